# Optimizing a Trainium2 kernel written in Bass

```python
import jax, jax.numpy as jnp
from jax import lax
import numpy as np

D_MODEL = 1024
BATCH = 8
SEQ = 4096
DEPTH = 1

CHUNK = 64
Q_BLOCK = 128
SB_WIDTH = D_MODEL // 2
SB_HEAD_DIM = 64
SB_HEADS = SB_WIDTH // SB_HEAD_DIM
ML_WIDTH = D_MODEL - SB_WIDTH
ML_HEADS = 4
ML_HEAD_DIM = ML_WIDTH // ML_HEADS
MIX_WIDTH = SB_WIDTH + ML_WIDTH
CONV_WIDTH = 4
IN_SIZES = (SB_WIDTH, SB_WIDTH, SB_WIDTH, 2 * ML_WIDTH, ML_WIDTH, ML_WIDTH, ML_HEADS, ML_HEADS)
IN_WIDTH = 3 * SB_WIDTH + 4 * ML_WIDTH + 2 * ML_HEADS
PEER_HEADS = 8
PEER_NKEYS = 128
PEER_EXPERTS = PEER_NKEYS * PEER_NKEYS
PEER_TOPK = 16
PEER_QDIM = 256
PEER_HALF = PEER_QDIM // 2
PEER_TOKEN_BLOCK = 128
N_MOD = 6
EPS = 1e-6

kernel_name = "hybrid_stickbreak_mlstm_peer_adaln"


def rms_norm(x, g):
    x32 = x.astype(jnp.float32)
    y = x32 * lax.rsqrt(jnp.mean(x32 * x32, axis=-1, keepdims=True) + EPS)
    return (y * g.astype(jnp.float32)).astype(x.dtype)


def causal_depthwise_conv(x, w):
    return lax.conv_general_dilated(
        x, w[:, None, :].astype(x.dtype), window_strides=(1,),
        padding=[(CONV_WIDTH - 1, 0)], dimension_numbers=('NWC', 'WIO', 'NWC'),
        feature_group_count=x.shape[-1])


def stick_breaking(q, k, v):
    s_len, d = q.shape[2], q.shape[-1]
    scale = d ** -0.5
    outs = []
    for t0 in range(0, s_len, Q_BLOCK):
        t1 = t0 + Q_BLOCK
        z = jnp.einsum('bhtd,bhsd->bhts', q[:, :, t0:t1], k[:, :, :t1]).astype(jnp.float32) * scale
        visible = jnp.arange(t1)[None, :] < jnp.arange(t0, t1)[:, None]
        log_keep = jnp.where(visible, jax.nn.log_sigmoid(-z), 0.0)
        log_between = lax.cumsum(log_keep, axis=3, reverse=True) - log_keep
        w = jnp.where(visible, jnp.exp(jax.nn.log_sigmoid(z) + log_between), 0.0)
        outs.append(jnp.einsum('bhts,bhsd->bhtd', w, v[:, :, :t1].astype(jnp.float32)))
    return jnp.concatenate(outs, axis=2).astype(v.dtype)


def mlstm(q, k, v, i_pre, f_pre):
    b, nh, s_len, d = q.shape
    nc = s_len // CHUNK
    f32 = jnp.float32

    def to_chunks(t):
        t = t.reshape((b, nh, nc, CHUNK) + t.shape[3:])
        return jnp.moveaxis(t, 2, 0)

    qc = to_chunks(q.astype(f32))
    kc = to_chunks(k.astype(f32) * (d ** -0.5))
    vc = to_chunks(v.astype(f32))
    a = jnp.cumsum(to_chunks(jax.nn.log_sigmoid(f_pre.astype(f32))), axis=-1)
    li = to_chunks(i_pre.astype(f32))
    causal = jnp.tril(jnp.ones((CHUNK, CHUNK), dtype=bool))

    def step(carry, inp):
        c_state, n_state, m_state = carry
        q_c, k_c, v_c, a_c, i_c = inp
        log_d = jnp.where(causal, a_c[..., :, None] - a_c[..., None, :] + i_c[..., None, :], -jnp.inf)
        log_inter = a_c + m_state[..., None]
        m_row = jnp.maximum(jnp.max(log_d, axis=-1), log_inter)
        w_intra = jnp.exp(log_d - m_row[..., None])
        w_inter = jnp.exp(log_inter - m_row)
        s_qk = jnp.einsum('bhtk,bhsk->bhts', q_c, k_c) * w_intra
        num = (jnp.einsum('bhts,bhsv->bhtv', s_qk, v_c)
               + w_inter[..., None] * jnp.einsum('bhvk,bhtk->bhtv', c_state, q_c))
        den = jnp.sum(s_qk, axis=-1) + w_inter * jnp.einsum('bhk,bhtk->bht', n_state, q_c)
        h = num / jnp.maximum(jnp.abs(den), jnp.exp(-m_row))[..., None]
        a_end = a_c[..., -1]
        log_w = a_end[..., None] - a_c + i_c
        m_new = jnp.maximum(a_end + m_state, jnp.max(log_w, axis=-1))
        w_s = jnp.exp(log_w - m_new[..., None])
        decay = jnp.exp(a_end + m_state - m_new)
        c_new = decay[..., None, None] * c_state + jnp.einsum('bhs,bhsv,bhsk->bhvk', w_s, v_c, k_c)
        n_new = decay[..., None] * n_state + jnp.einsum('bhs,bhsk->bhk', w_s, k_c)
        return (c_new, n_new, m_new), h

    init = (jnp.zeros((b, nh, d, d), f32), jnp.zeros((b, nh, d), f32), jnp.zeros((b, nh), f32))
    _, h = lax.scan(step, init, (qc, kc, vc, a, li))
    h = jnp.moveaxis(h, 0, 2).reshape(b, nh, s_len, d)
    return h.astype(v.dtype)


def peer(h, w_q, sub_keys, expert_u, expert_v):
    b, s_len, d = h.shape
    q = jnp.einsum('bsd,dq->bsq', h, w_q).reshape(b, s_len, PEER_HEADS, 2, PEER_HALF)
    scores = jnp.einsum('bshpc,hpnc->bshpn', q, sub_keys).astype(jnp.float32)
    top_s, top_i = lax.top_k(scores, PEER_TOPK)
    cand_s = (top_s[..., 0, :, None] + top_s[..., 1, None, :]).reshape(b, s_len, PEER_HEADS, PEER_TOPK * PEER_TOPK)
    cand_i = (top_i[..., 0, :, None] * PEER_NKEYS + top_i[..., 1, None, :]).reshape(b, s_len, PEER_HEADS, PEER_TOPK * PEER_TOPK)
    best_s, best_pos = lax.top_k(cand_s, PEER_TOPK)
    best_i = jnp.take_along_axis(cand_i, best_pos, axis=-1)
    gate = jax.nn.softmax(best_s, axis=-1)
    n_sel = PEER_HEADS * PEER_TOPK
    nb = (b * s_len) // PEER_TOKEN_BLOCK
    xs = h.reshape(nb, PEER_TOKEN_BLOCK, d)
    idx = best_i.reshape(nb, PEER_TOKEN_BLOCK, n_sel)
    gs = gate.reshape(nb, PEER_TOKEN_BLOCK, n_sel)

    def token_block(args):
        xb, ib, gb = args
        u = jnp.take(expert_u, ib, axis=0)
        act = jax.nn.gelu(jnp.einsum('td,tkd->tk', xb, u).astype(jnp.float32), approximate=False)
        vv = jnp.take(expert_v, ib, axis=0)
        return jnp.einsum('tk,tkd->td', (gb * act).astype(h.dtype), vv)

    y = lax.map(token_block, (xs, idx, gs))
    return y.reshape(b, s_len, d)


def setup_inputs(seed: int = 0) -> dict:
    key = jax.random.key(seed)
    ks = jax.random.split(key, 20)
    nrm = jax.random.normal
    D = D_MODEL
    return {
        "x": nrm(ks[0], (BATCH, SEQ, D), jnp.float32),
        "c": nrm(ks[1], (BATCH, D), jnp.float32),
        "w_ada": nrm(ks[2], (DEPTH, D, N_MOD * D), jnp.float32) * (0.5 * D ** -0.5),
        "b_ada": nrm(ks[3], (DEPTH, N_MOD * D), jnp.float32) * 0.02,
        "g_norm1": 1.0 + 0.02 * nrm(ks[4], (DEPTH, D), jnp.float32),
        "w_in": nrm(ks[5], (DEPTH, D, IN_WIDTH), jnp.float32) * D ** -0.5,
        "b_igate": 0.1 * nrm(ks[6], (DEPTH, ML_HEADS), jnp.float32),
        "b_fgate": 3.0 + 3.0 * jax.random.uniform(ks[7], (DEPTH, ML_HEADS), jnp.float32),
        "conv_w": nrm(ks[8], (DEPTH, CONV_WIDTH, 2 * ML_WIDTH), jnp.float32) * CONV_WIDTH ** -0.5,
        "g_q_sb": 1.0 + 0.02 * nrm(ks[9], (DEPTH, SB_HEAD_DIM), jnp.float32),
        "g_k_sb": 1.0 + 0.02 * nrm(ks[10], (DEPTH, SB_HEAD_DIM), jnp.float32),
        "g_out_sb": 1.0 + 0.02 * nrm(ks[11], (DEPTH, SB_HEADS, SB_HEAD_DIM), jnp.float32),
        "g_out_ml": 1.0 + 0.02 * nrm(ks[12], (DEPTH, ML_HEADS, ML_HEAD_DIM), jnp.float32),
        "w_out": nrm(ks[13], (DEPTH, MIX_WIDTH, D), jnp.float32) * MIX_WIDTH ** -0.5,
        "g_norm2": 1.0 + 0.02 * nrm(ks[14], (DEPTH, D), jnp.float32),
        "w_q_peer": nrm(ks[15], (DEPTH, D, PEER_HEADS * PEER_QDIM), jnp.float32) * D ** -0.5,
        "sub_keys": nrm(ks[16], (DEPTH, PEER_HEADS, 2, PEER_NKEYS, PEER_HALF), jnp.float32) * PEER_HALF ** -0.5,
        "expert_u": nrm(ks[17], (DEPTH, PEER_EXPERTS, D), jnp.float32) * D ** -0.5,
        "expert_v": nrm(ks[18], (DEPTH, PEER_EXPERTS, D), jnp.float32),
    }


def reference(x, c, w_ada, b_ada, g_norm1, w_in, b_igate, b_fgate, conv_w, g_q_sb, g_k_sb,
              g_out_sb, g_out_ml, w_out, g_norm2, w_q_peer, sub_keys, expert_u, expert_v):
    b, s_len, _ = x.shape
    cuts = []
    acc = 0
    for size in IN_SIZES[:-1]:
        acc += size
        cuts.append(acc)

    def to_heads(t, n):
        return t.reshape(b, s_len, n, -1).transpose(0, 2, 1, 3)

    for l in range(DEPTH):
        mod = jnp.einsum('bd,dm->bm', jax.nn.silu(c), w_ada[l]) + b_ada[l]
        shift1, scale1, gate1, shift2, scale2, gate2 = jnp.split(mod[:, None, :], N_MOD, axis=-1)

        h = rms_norm(x, g_norm1[l]) * (1 + scale1) + shift1
        proj = jnp.einsum('bsd,dn->bsn', h, w_in[l])
        q_sb, k_sb, v_sb, qk_ml, v_ml, o_ml, i_ml, f_ml = jnp.split(proj, cuts, axis=-1)

        q_sb = rms_norm(to_heads(q_sb, SB_HEADS), g_q_sb[l])
        k_sb = rms_norm(to_heads(k_sb, SB_HEADS), g_k_sb[l])
        y_sb = stick_breaking(q_sb, k_sb, to_heads(v_sb, SB_HEADS)).transpose(0, 2, 1, 3)
        y_sb = rms_norm(y_sb, g_out_sb[l])

        qk_ml = jax.nn.silu(causal_depthwise_conv(qk_ml, conv_w[l]))
        q_ml, k_ml = jnp.split(qk_ml, 2, axis=-1)
        i_pre = (i_ml + b_igate[l]).transpose(0, 2, 1)
        f_pre = (f_ml + b_fgate[l]).transpose(0, 2, 1)
        y_ml = mlstm(to_heads(q_ml, ML_HEADS), to_heads(k_ml, ML_HEADS), to_heads(v_ml, ML_HEADS), i_pre, f_pre)
        y_ml = rms_norm(y_ml.transpose(0, 2, 1, 3), g_out_ml[l]) * jax.nn.sigmoid(o_ml).reshape(b, s_len, ML_HEADS, ML_HEAD_DIM)

        mixed = jnp.concatenate([y_sb.reshape(b, s_len, SB_WIDTH), y_ml.reshape(b, s_len, ML_WIDTH)], axis=-1)
        x = x + gate1 * jnp.einsum('bsm,md->bsd', mixed, w_out[l])

        h2 = rms_norm(x, g_norm2[l]) * (1 + scale2) + shift2
        x = x + gate2 * peer(h2, w_q_peer[l], sub_keys[l], expert_u[l], expert_v[l])
    return x
```

```python
from contextlib import ExitStack
import numpy as np
import concourse.bass as bass
import concourse.mybir as mybir
from concourse.bass_utils import run_bass_kernel_spmd

F32 = mybir.dt.float32
BF16 = mybir.dt.bfloat16
U32 = mybir.dt.uint32
I32 = mybir.dt.int32
ALU = mybir.AluOpType
AF = mybir.ActivationFunctionType
AX = mybir.AxisListType


class Sync:
    def __init__(self, nc):
        self.nc = nc
        self.es = ExitStack()
        self.eng = {'pe': nc.tensor, 'act': nc.scalar, 'dve': nc.vector,
                    'pool': nc.gpsimd, 'sp': nc.sync}
        self.sem = {}
        self.cnt = {}
        self.seen = {k: {} for k in self.eng}
        self.lastw = {}
        self.readers = {}
        self.nsem = 0
        self.ninst = 0

    def __enter__(self):
        self.es.__enter__()
        for k in self.eng:
            self._mksem(k)
        return self

    def __exit__(self, *a):
        return self.es.__exit__(*a)

    def _mksem(self, key):
        if key not in self.sem:
            self.nsem += 1
            self.sem[key] = self.es.enter_context(self.nc.semaphore("s%d" % self.nsem))
            self.cnt[key] = 0
        return self.sem[key]

    def sb(self, name, shape, dtype, stack=None):
        self.nalloc = getattr(self, 'nalloc', 0) + 1
        return (stack or self.es).enter_context(
            self.nc.sbuf_tensor("sb%d_%s" % (self.nalloc, name), list(shape), dtype))

    def ps(self, name, shape, dtype, stack=None):
        self.nalloc = getattr(self, 'nalloc', 0) + 1
        return (stack or self.es).enter_context(
            self.nc.psum_tensor("pp%d_%s" % (self.nalloc, name), list(shape), dtype))

    def _waits(self, e, reads, writes, is_dma):
        need = {}

        def add(rec, raw):
            key, val, src = rec
            if (not is_dma) and src == e:
                if e == 'pe':
                    return
            if self.seen[e].get(key, 0) >= val:
                return
            if need.get(key, 0) < val:
                need[key] = val

        for b in reads:
            w = self.lastw.get(b)
            if w:
                add(w, True)
        for b in writes:
            w = self.lastw.get(b)
            if w:
                add(w, False)
            for key, (val, src) in self.readers.get(b, {}).items():
                add((key, val, src), False)
        for key, val in need.items():
            self.eng[e].wait_ge(self.sem[key], val)
            self.seen[e][key] = val

    def _record(self, rec, reads, writes):
        key, val, src = rec
        for b in reads:
            d = self.readers.setdefault(b, {})
            d[key] = (val, src)
        for b in writes:
            self.lastw[b] = rec
            self.readers[b] = {}

    def op(self, e, fn, reads=(), writes=()):
        self._waits(e, reads, writes, False)
        ins = fn(self.eng[e])
        self.cnt[e] += 1
        ins.then_inc(self.sem[e], 1)
        self.ninst += 1
        self._record((e, self.cnt[e], e), reads, writes)
        return ins

    def dma(self, q, out, in_, reads=(), writes=(), semkey=None, fn=None, **kw):
        assert semkey is not None
        key = ('dma', semkey)
        self._mksem(key)
        self._waits(q, reads, writes, True)
        if fn is not None:
            ins = fn(self.eng[q])
        else:
            ins = self.eng[q].dma_start(out=out, in_=in_, **kw)
        self.cnt[key] += 16
        ins.then_inc(self.sem[key], 16)
        self.ninst += 1
        self._record((key, self.cnt[key], 'dma'), reads, writes)
        return ins

    def finish(self, bufs):
        self._waits('sp', bufs, (), True)


D = 1024
NIN = 3592
EPS = 1e-6
NEXP = 16384


class Ring:
    def __init__(self, S, name, n, shape, dtype, st, psum=False):
        self.tiles = []
        for i in range(n):
            nm = "%s%d" % (name, i)
            t = S.ps(nm, shape, dtype, st) if psum else S.sb(nm, shape, dtype, st)
            self.tiles.append((t, (name, i)))
        self.i = 0

    def next(self):
        r = self.tiles[self.i % len(self.tiles)]
        self.i += 1
        return r


def _barrier(S):
    excl = getattr(S, 'barrier_exclude', ())
    for e in S.eng:
        for key, sem in S.sem.items():
            if key in excl:
                continue
            v = S.cnt[key]
            if v > 0 and S.seen[e].get(key, 0) < v and key != e:
                S.eng[e].wait_ge(sem, v)
                S.seen[e][key] = v


def _declare(nc, Slen, debug):
    T = {}

    def inp(name, shape, dt=F32):
        T[name] = nc.dram_tensor(name, list(shape), dt, kind="ExternalInput")

    def scr(name, shape, dt):
        T[name] = nc.dram_tensor(name, list(shape), dt,
                                 kind="ExternalOutput" if debug else "Internal")

    inp("x", [Slen, D]); inp("c_t", [128, 8]); inp("w_ada", [D, 6 * D]); inp("b_ada_bc", [128, 6 * D])
    inp("g1_bc", [128, D]); inp("g2_bc", [128, D]); inp("w_in", [D, NIN]); inp("bgate_bc", [128, 8])
    inp("convw", [128, 8, 4]); inp("gq_col", [128, 1]); inp("gk_col", [128, 1]); inp("gsb_col", [128, 4])
    inp("gml_bc", [128, 512]); inp("w_out", [D, D]); inp("w_q", [D, 2048]); inp("keysT", [128, 16, 128])
    inp("expert_uv", [NEXP, 2 * D])
    T["uvb"] = nc.dram_tensor("uvb", [NEXP, 2 * D], BF16, kind="Internal")
    T["out"] = nc.dram_tensor("out", [Slen, D], F32, kind="ExternalOutput")
    scr("qsbT", [512, Slen], BF16); scr("ksbT", [512, Slen], BF16); scr("vsb", [Slen, 1024], BF16)
    scr("qmlT", [512, Slen], BF16); scr("kmlT", [512, Slen], BF16); scr("vml", [Slen, 512], BF16)
    scr("sigo", [Slen, 512], BF16); scr("gates", [Slen, 8], F32); scr("mixedT", [D, Slen], BF16)
    scr("x1", [Slen, D], F32)
    if debug:
        scr("dbg_mod", [128, 6 * D], F32)
    return T


def _consts(S, P):
    nc = S.nc
    es = S.es
    ones = S.sb("ones", [128, 128], F32); P['ones'] = ones
    S.op('pool', lambda e: e.memset(ones[:, :], 1.0), writes=['ones'])
    neg = S.sb("negones", [128, 128], F32)
    S.op('pool', lambda e: e.memset(neg[:, :], -1.0), writes=['neg'])

    def sel(name, src, srck, cmp, step, cm, dt=None):
        t = S.sb(name, [128, 128], F32)
        S.op('pool', lambda e: e.affine_select(out=t[:, :], in_=src[:, :], pattern=[[step, 128]],
                                               compare_op=cmp, fill=0.0, base=0, channel_multiplier=cm),
             reads=[srck], writes=[name])
        P[name] = t
        if dt is not None:
            tb = S.sb(name + "_b", [128, 128], dt)
            S.op('dve', lambda e: e.tensor_copy(out=tb[:, :], in_=t[:, :]), reads=[name], writes=[name + "_b"])
            P[name + "_b"] = tb
        return t

    sel("ident", ones, 'ones', ALU.is_equal, 1, -1, BF16)
    sel("tri", ones, 'ones', ALU.is_ge, 1, -1)
    sel("strict", ones, 'ones', ALU.is_gt, 1, -1, BF16)
    sel("cumU", neg, 'neg', ALU.is_gt, -1, 1, BF16)
    sel("cumL", neg, 'neg', ALU.is_ge, 1, -1, BF16)
    bo = S.sb("blockones", [128, 128], BF16); P['blockones'] = bo
    S.op('pool', lambda e: e.memset(bo[:, :], 0.0), writes=['blockones'])
    S.op('pool', lambda e: e.memset(bo[0:64, 0:64], 1.0), reads=['blockones'], writes=['blockones'])
    S.op('pool', lambda e: e.memset(bo[64:128, 64:128], 1.0), reads=['blockones'], writes=['blockones'])
    P['ps'] = [(S.ps("ps%d" % i, [128, 512], F32), ('ps', i)) for i in range(7)]
    P['psb'] = S.ps("psb", [128, 1024], BF16)
    P['ps_all'] = list(P['ps'])
    P['psi'] = 0
    P['mod'] = S.sb("mod", [128, 6 * D], F32)


def _psnext(P):
    r = P['ps'][P['psi'] % len(P['ps'])]
    P['psi'] += 1
    return r


def _preload(S, T, P):
    w_q = S.sb('w_q', [128, 8, 2048], BF16)
    wv = T['w_q'].ap().rearrange("(j p) n -> p j n", p=128)
    WQ = []
    keys = S.sb('keysb', [128, 16, 128], BF16)
    KEYS = []
    P['w_q'], P['WQ'], P['keys'], P['KEYS'] = w_q, WQ, keys, KEYS

    def issue_late():
        for j in range(8):
            for hf in range(2):
                k = ('w_q', j, hf)
                WQ.append(k)
                S.dma('pool', w_q[:, j, hf * 1024:(hf + 1) * 1024], wv[:, j, hf * 1024:(hf + 1) * 1024],
                      writes=[k], semkey='w_q')
        for q in range(4):
            KEYS.append(('keys', q))
            S.dma('pool', keys[:, q * 4:(q + 1) * 4, :], T['keysT'][:, q * 4:(q + 1) * 4, :], writes=[('keys', q)],
                  semkey='keys')
    P['issue_late'] = issue_late


def _load_w_in(S, T, P, st):
    w_in = S.sb('w_in', [128, 8, NIN], BF16, st)
    w_in_v = T['w_in'].ap().rearrange("(j p) n -> p j n", p=128)
    WIN = []
    for j in range(8):
        for q in range(4):
            k = ('w_in', j, q)
            WIN.append(k)
            S.dma('pool', w_in[:, j, q * 898:(q + 1) * 898], w_in_v[:, j, q * 898:(q + 1) * 898],
                  writes=[k], semkey='w_in')
    P['w_in'], P['WIN'] = w_in, WIN


def phase_A(S, T, P):
    ones = P['ones']
    mod = P['mod']
    with ExitStack() as st:
        cT = S.sb('cT', [128, 8], F32, st)
        sc = S.sb('sc', [128, 8], F32, st)
        Lc = S.sb('Lc', [128, 8, 128], F32, st)
        wb = [S.sb('wada%d' % i, [128, 3072], F32, st) for i in range(2)]
        bada = S.sb('bada', [128, 3072], F32, st)
        gbc = S.sb('gbc', [128, 2, D], F32, st)
        S.dma('sp', cT[:, :], T['c_t'][:, :], writes=['cT'], semkey='cT')
        S.dma('sp', gbc[:, 0, :], T['g1_bc'][:, :], writes=['gbc0'], semkey='gbc0')
        S.dma('sp', gbc[:, 1, :], T['g2_bc'][:, :], writes=['gbc1'], semkey='gbc1')
        S.op('act', lambda e: e.activation(out=sc[:, :], in_=cT[:, :], func=AF.Silu), reads=['cT'], writes=['sc'])
        for j in range(8):
            S.op('dve', lambda e, j=j: e.tensor_scalar(out=Lc[:, j, :], in0=ones[:, :], scalar1=sc[:, j:j + 1],
                                                       scalar2=None, op0=ALU.mult),
                 reads=['ones', 'sc'], writes=[('Lc', j)])
        for half in range(2):
            c0 = half * 3072
            S.dma('sp', bada[:, :], T['b_ada_bc'][:, c0:c0 + 3072], writes=['bada'], semkey='bada')
            for j in range(8):
                slot = j % 2
                S.dma('sp', wb[slot][:, :], T['w_ada'][j * 128:(j + 1) * 128, c0:c0 + 3072],
                      writes=[('wb', slot)], semkey=('wb', slot))
                for n in range(6):
                    S.op('pe', lambda e, j=j, n=n, slot=slot: e.matmul(
                        P['ps'][n][0][:, :], lhsT=Lc[:, j, :], rhs=wb[slot][:, n * 512:(n + 1) * 512],
                        start=(j == 0), stop=(j == 7)),
                        reads=[('wb', slot), ('Lc', j)], writes=[('ps', n)])
            for n in range(6):
                S.op('dve', lambda e, n=n: e.tensor_tensor(
                    out=mod[:, c0 + n * 512:c0 + (n + 1) * 512], in0=P['ps'][n][0][:, :],
                    in1=bada[:, n * 512:(n + 1) * 512], op=ALU.add),
                    reads=[('ps', n), 'bada'], writes=['mod'])
        if 'dbg_mod' in T:
            S.dma('sp', T['dbg_mod'][:, :], mod[:, :], reads=['mod'], semkey='dbgmod')
        for i, c0 in ((0, 1024), (1, 4096)):
            S.op('dve', lambda e, i=i, c0=c0: e.scalar_tensor_tensor(
                out=mod[:, c0:c0 + D], in0=mod[:, c0:c0 + D], scalar=1.0, in1=gbc[:, i, :],
                op0=ALU.add, op1=ALU.mult), reads=['mod', 'gbc%d' % i], writes=['mod'])
        _barrier(S)


def phase_B1(S, T, P, Slen):
    mod = P['mod']
    ng = Slen // 512
    identb = P['ident_b']
    psb = P['psb']
    with ExitStack() as st:
        w_in, WIN = P['w_in'], P['WIN']
        cw = S.sb('cw', [128, 8, 4], F32, st)
        gq = S.sb('gq', [128, 2], F32, st)
        bg = S.sb('bg', [128, 8], F32, st)
        S.dma('sp', cw[:, :, :], T['convw'][:, :, :], writes=['cw'], semkey='cw')
        S.dma('sp', gq[:, 0:1], T['gq_col'][:, :], writes=['gq0'], semkey='gq0')
        S.dma('sp', gq[:, 1:2], T['gk_col'][:, :], writes=['gq1'], semkey='gq1')
        S.dma('sp', bg[:, :], T['bgate_bc'][:, :], writes=['bg'], semkey='bg')
        xr = Ring(S, 'xt', 2, [128, D], F32, st)
        hbr = Ring(S, 'hb', 2, [128, D], BF16, st)
        junk = S.sb('junkb', [128, D], BF16, st)
        ssr = Ring(S, 'ss', 2, [128, 1], F32, st)
        tmp32 = S.sb('tmp32', [128, D], F32, st)
        hTs = [S.sb('hT%d' % i_, [128, 8, 512], BF16, st) for i_ in range(2)]
        sqr = Ring(S, 'sq', 2, [128, 512], BF16, st)
        rrr = Ring(S, 'rr', 2, [128, 512], F32, st)
        qnr = Ring(S, 'qn', 2, [128, 512], BF16, st)
        cb = [S.sb('cb%d' % j, [128, 515], F32, st) for j in range(8)]
        accr = Ring(S, 'acc', 3, [128, 512], F32, st)
        sor = Ring(S, 'so', 2, [128, 512], BF16, st)
        tokr = Ring(S, 'tok', 3, [128, 512], BF16, st)
        vpr = Ring(S, 'vpad', 2, [128, 1024], BF16, st)
        for t_, k_ in vpr.tiles:
            S.op('pool', lambda e, t_=t_: e.memset(t_[:, :], 0.0), writes=[k_])
        gr = Ring(S, 'gt', 2, [128, 8], F32, st)
        etr = Ring(S, 'et', 2, [128, 4], F32, st)
        for j in range(8):
            S.op('pool', lambda e, j=j: e.memset(cb[j][:, 0:3], 0.0), writes=[('cb', j)])
        conv_tail = []
        norm_tail = []
        def prologue(g):
            hT = hTs[g % 2]
            for tl in range(4):
                t0 = g * 512 + tl * 128
                xt, xk = xr.next()
                S.dma('sp', xt[:, :], T['x'][t0:t0 + 128, :], writes=[xk], semkey=xk)
                ss, sk = ssr.next()
                S.op('act', lambda e: e.activation(out=junk[:, :], in_=xt[:, :], func=AF.Square, accum_out=ss[:, :]),
                     reads=[xk], writes=['junkb', sk])
                S.op('act', lambda e: e.activation(out=ss[:, :], in_=ss[:, :], func=AF.Ln, scale=1.0 / D, bias=EPS),
                     reads=[sk], writes=[sk])
                S.op('act', lambda e: e.activation(out=ss[:, :], in_=ss[:, :], func=AF.Exp, scale=-0.5),
                     reads=[sk], writes=[sk])
                S.op('dve', lambda e: e.scalar_tensor_tensor(out=tmp32[:, :], in0=xt[:, :], scalar=ss[:, 0:1],
                                                             in1=mod[:, 1024:2048], op0=ALU.mult, op1=ALU.mult),
                     reads=[xk, sk, 'mod'], writes=['tmp32'])
                hb, hk = hbr.next()
                S.op('dve', lambda e: e.tensor_tensor(out=hb[:, :], in0=tmp32[:, :], in1=mod[:, 0:1024], op=ALU.add),
                     reads=['tmp32', 'mod'], writes=[hk])
                for j in range(8):
                    S.op('pe', lambda e, j=j: e.transpose(out=psb[:, j * 128:(j + 1) * 128],
                                                          in_=hb[:, j * 128:(j + 1) * 128], identity=identb[:, :]),
                         reads=[hk, 'ident_b'], writes=['psb'])
                S.op('act', lambda e: e.activation(out=hT[:, :, tl * 128:(tl + 1) * 128],
                                                   in_=psb[:, :].rearrange("p (j t) -> p j t", j=8), func=AF.Copy),
                     reads=['psb'], writes=[('hT', g % 2, tl)])

        prologue(0)
        for g in range(ng):
            gs = slice(g * 512, (g + 1) * 512)
            hT = hTs[g % 2]
            HT = [('hT', g % 2, i_) for i_ in range(4)]
            for m in range(16):
                col0 = m * 128 if m < 8 else 1536 + (m - 8) * 128
                pst, pk = _psnext(P)
                for j in range(8):
                    S.op('pe', lambda e, j=j: e.matmul(pst[:, :], lhsT=w_in[:, j, col0:col0 + 128], rhs=hT[:, j, :],
                                                       start=(j == 0), stop=(j == 7)),
                         reads=WIN + HT, writes=[pk])
                if m < 8:
                    sq, sqk = sqr.next()
                    S.op('act', lambda e: e.activation(out=sq[:, :], in_=pst[:, :], func=AF.Square),
                         reads=[pk], writes=[sqk])
                    while norm_tail:
                        norm_tail.pop(0)()

                    def ntail(m=m, pst=pst, pk=pk, sq=sq, sqk=sqk):
                        ps2, pk2 = _psnext(P)
                        S.op('pe', lambda e: e.matmul(ps2[:, :], lhsT=P['blockones'][:, :], rhs=sq[:, :],
                                                      start=True, stop=True),
                             reads=[sqk, 'blockones'], writes=[pk2])
                        rr, rk = rrr.next()
                        S.op('act', lambda e: e.activation(out=rr[:, :], in_=ps2[:, :], func=AF.Ln, scale=1.0 / 64, bias=EPS),
                             reads=[pk2], writes=[rk])
                        S.op('act', lambda e: e.activation(out=rr[:, :], in_=rr[:, :], func=AF.Exp, scale=-0.5,
                                                           bias=(float(np.log(0.125)) if m < 4 else 0.0)),
                             reads=[rk], writes=[rk])
                        qn, qk = qnr.next()
                        gi = 0 if m < 4 else 1
                        S.op('dve', lambda e: e.scalar_tensor_tensor(out=qn[:, :], in0=pst[:, :], scalar=gq[:, gi:gi + 1],
                                                                     in1=rr[:, :], op0=ALU.mult, op1=ALU.mult),
                             reads=[pk, rk, 'gq%d' % gi], writes=[qk])
                        dst = T['qsbT'] if m < 4 else T['ksbT']
                        r0 = (m % 4) * 128
                        S.dma('sp', dst[r0:r0 + 128, gs], qn[:, :], reads=[qk], semkey=qk)
                    norm_tail.append(ntail)
                else:
                    j = m - 8
                    S.op('act', lambda e: e.activation(out=cb[j][:, 3:515], in_=pst[:, :], func=AF.Copy),
                         reads=[pk], writes=[('cb', j)])
                    while norm_tail:
                        norm_tail.pop(0)()
                    while conv_tail:
                        conv_tail.pop(0)()
                    at, ak = accr.next()
                    S.op('dve', lambda e: e.tensor_scalar(out=at[:, :], in0=cb[j][:, 3:515], scalar1=cw[:, j, 3:4],
                                                          scalar2=None, op0=ALU.mult),
                         reads=[('cb', j), 'cw'], writes=[ak])
                    for tap in (2, 1, 0):
                        S.op('dve', lambda e, tap=tap: e.scalar_tensor_tensor(
                            out=at[:, :], in0=cb[j][:, tap:tap + 512], scalar=cw[:, j, tap:tap + 1], in1=at[:, :],
                            op0=ALU.mult, op1=ALU.add), reads=[('cb', j), ak, 'cw'], writes=[ak])
                    S.op('pool', lambda e: e.tensor_copy(out=cb[j][:, 0:3], in_=cb[j][:, 512:515]),
                         reads=[('cb', j)], writes=[('cb', j)])
                    def tail(j=j, at=at, ak=ak):
                        so, sok = sor.next()
                        S.op('act', lambda e: e.activation(out=so[:, :], in_=at[:, :], func=AF.Silu),
                             reads=[ak], writes=[sok])
                        dst = T['qmlT'] if j < 4 else T['kmlT']
                        r0 = (j % 4) * 128
                        S.dma('sp', dst[r0:r0 + 128, gs], so[:, :], reads=[sok], semkey=sok)
                    conv_tail.append(tail)
            while conv_tail:
                conv_tail.pop(0)()
            if g + 1 < ng:
                prologue(g + 1)
            for tl in range(4):
                t0 = g * 512 + tl * 128
                for c0, name in ((1024, 'vsb'), (2560, 'vml'), (3072, 'sigo')):
                    pst, pk = _psnext(P)
                    for j in range(8):
                        S.op('pe', lambda e, j=j: e.matmul(pst[:, :], lhsT=hT[:, j, tl * 128:(tl + 1) * 128],
                                                           rhs=w_in[:, j, c0:c0 + 512], start=(j == 0), stop=(j == 7)),
                             reads=WIN + [('hT', g % 2, tl)], writes=[pk])
                    if name == 'vsb':
                        vp, vpk = vpr.next()
                        vp4 = vp[:, :].rearrange("p (pr two c) -> p pr two c", two=2, c=128)
                        ps4 = pst[:, :].rearrange("p (pr two d) -> p pr two d", two=2, d=64)
                        S.op('act', lambda e: e.activation(out=vp4[:, :, 0, 0:64], in_=ps4[:, :, 0, :], func=AF.Copy),
                             reads=[pk], writes=[vpk])
                        S.op('act', lambda e: e.activation(out=vp4[:, :, 1, 64:128], in_=ps4[:, :, 1, :], func=AF.Copy),
                             reads=[pk, vpk], writes=[vpk])
                        S.dma('sp', T['vsb'][t0:t0 + 128, :], vp[:, :], reads=[vpk], semkey=vpk)
                        continue
                    tt, tk = tokr.next()
                    S.op('act', lambda e: e.activation(out=tt[:, :], in_=pst[:, :],
                                                       func=(AF.Sigmoid if name == 'sigo' else AF.Copy)),
                         reads=[pk], writes=[tk])
                    S.dma('sp', T[name][t0:t0 + 128, :], tt[:, :], reads=[tk], semkey=tk)
                pst, pk = _psnext(P)
                for j in range(8):
                    S.op('pe', lambda e, j=j: e.matmul(pst[:, 0:8], lhsT=hT[:, j, tl * 128:(tl + 1) * 128],
                                                       rhs=w_in[:, j, 3584:3592], start=(j == 0), stop=(j == 7)),
                         reads=WIN + [('hT', g % 2, tl)], writes=[pk])
                gt, gk = gr.next()
                S.op('dve', lambda e: e.tensor_tensor(out=gt[:, :], in0=pst[:, 0:8], in1=bg[:, :], op=ALU.add),
                     reads=[pk, 'bg'], writes=[gk])
                et, ek = etr.next()
                S.op('act', lambda e: e.activation(out=et[:, :], in_=gt[:, 4:8], func=AF.Exp, scale=-1.0),
                     reads=[gk], writes=[ek])
                S.op('act', lambda e: e.activation(out=et[:, :], in_=et[:, :], func=AF.Ln, bias=1.0),
                     reads=[ek], writes=[ek])
                S.op('dve', lambda e: e.tensor_scalar(out=gt[:, 4:8], in0=et[:, :], scalar1=-1.0, scalar2=None,
                                                      op0=ALU.mult), reads=[ek, gk], writes=[gk])
                S.dma('sp', T['gates'][t0:t0 + 128, :], gt[:, :], reads=[gk], semkey=gk)
        _barrier(S)


def build(Slen=4096, debug=False, phases="AB"):
    nc = bass.Bass("TRN2", target_bir_lowering=False)
    T = _declare(nc, Slen, debug)
    S = Sync(nc)
    P = {}
    with S:
        _consts(S, P)
        _preload(S, T, P)
        st_win = ExitStack()
        _load_w_in(S, T, P, st_win)
        P['issue_late']()
        with nc.named_scope("phA"):
            phase_A(S, T, P)
        def phase_B23(S, T, P, Slen):
            with ExitStack() as st3:
                phase_B2(S, T, P, Slen)
                _barrier(S)

        plan = [("B", phase_B1)]
        plan += [("2", phase_B2), ("3", phase_B3)]
        plan += [("4", phase_B4), ("C", phase_C)]
        for tag, fnp in plan:
            if all(t in phases for t in tag):
                with nc.named_scope("ph" + tag):
                    fnp(S, T, P, Slen)
            if tag == "B":
                st_win.close()
        _barrier(S)
        S.eng['sp'].wait_ge(S.sem['sp'], S.cnt['sp']) if S.cnt['sp'] else None
    return nc


def host_inputs(b, inputs, Slen=4096):
    f = lambda a: np.ascontiguousarray(a, dtype=np.float32)
    m = {}
    m["x"] = f(inputs["x"][b, :Slen])
    m["c_t"] = f(inputs["c"][b].reshape(8, 128).T)
    m["w_ada"] = f(inputs["w_ada"][0])
    m["b_ada_bc"] = f(np.broadcast_to(inputs["b_ada"][0][None, :], (128, 6 * D)))
    m["g1_bc"] = f(np.broadcast_to(inputs["g_norm1"][0][None, :], (128, D)))
    m["g2_bc"] = f(np.broadcast_to(inputs["g_norm2"][0][None, :], (128, D)))
    m["w_in"] = f(inputs["w_in"][0])
    bgate = np.concatenate([inputs["b_igate"][0], inputs["b_fgate"][0]])
    m["bgate_bc"] = f(np.broadcast_to(bgate[None, :], (128, 8)))
    m["convw"] = f(inputs["conv_w"][0].reshape(4, 8, 128).transpose(2, 1, 0))
    m["gq_col"] = f(np.tile(inputs["g_q_sb"][0], 2).reshape(128, 1))
    m["gk_col"] = f(np.tile(inputs["g_k_sb"][0], 2).reshape(128, 1))
    m["gsb_col"] = f(inputs["g_out_sb"][0].reshape(4, 128).T)
    m["gml_bc"] = f(np.broadcast_to(inputs["g_out_ml"][0].reshape(1, 512), (128, 512)))
    m["w_out"] = f(inputs["w_out"][0])
    m["w_q"] = f(inputs["w_q_peer"][0])
    m["keysT"] = f(inputs["sub_keys"][0].reshape(16, 128, 128).transpose(2, 0, 1))
    m["expert_uv"] = np.ascontiguousarray(
        np.concatenate([inputs["expert_u"][0], inputs["expert_v"][0]], axis=1), dtype=np.float32)
    return m


def _head_norm_T(S, P, pst, pk, gcol, gkey, out_tile, out_key, rrr, sqr, extra_bias=0.0):
    sq, sqk = sqr.next()
    S.op('act', lambda e: e.activation(out=sq[:, :], in_=pst[:, :], func=AF.Square), reads=[pk], writes=[sqk])
    ps2, pk2 = _psnext(P)
    S.op('pe', lambda e: e.matmul(ps2[:, :], lhsT=P['blockones'][:, :], rhs=sq[:, :], start=True, stop=True),
         reads=[sqk, 'blockones'], writes=[pk2])
    rr, rk = rrr.next()
    S.op('act', lambda e: e.activation(out=rr[:, :], in_=ps2[:, :], func=AF.Ln, scale=1.0 / 64, bias=EPS),
         reads=[pk2], writes=[rk])
    S.op('act', lambda e: e.activation(out=rr[:, :], in_=rr[:, :], func=AF.Exp, scale=-0.5, bias=extra_bias),
         reads=[rk], writes=[rk])
    S.op('dve', lambda e: e.scalar_tensor_tensor(out=out_tile[:, :], in0=pst[:, :], scalar=gcol, in1=rr[:, :],
                                                 op0=ALU.mult, op1=ALU.mult),
         reads=[pk, rk, gkey], writes=[out_key])


def phase_B2(S, T, P, Slen, other=None, every=7):
    ng = Slen // 512
    nt = Slen // 128
    with ExitStack() as st:
        kT = S.sb('kT', [128, 4, Slen], BF16, st)
        Vs = S.sb('Vs', [128, nt, 1024], BF16, st)
        gsb = S.sb('gsbc', [128, 4], F32, st)
        S.dma('sp', gsb[:, :], T['gsb_col'][:, :], writes=['gsb'], semkey='gsb')
        for pr in range(4):
            S.dma('sp', kT[:, pr, :], T['ksbT'][pr * 128:(pr + 1) * 128, :], writes=[('kT', pr)], semkey=('kT', pr))
        vview = T['vsb'].ap().rearrange("(n p) c -> p n c", p=128)
        for n0 in range(0, nt, 8):
            n1 = min(nt, n0 + 8)
            S.dma('sp', Vs[:, n0:n1, :], vview[:, n0:n1, :], writes=[('Vs', n0 // 8)], semkey=('Vs', n0 // 8))
        qr = Ring(S, 'qTg', 2, [128, 2, 512], BF16, st)
        for t_, k_ in qr.tiles:
            S.op('pool', lambda e, t_=t_: e.memset(t_[:, :, :], 0.0), writes=[k_])
        er = Ring(S, 'Esp', 6, [128, 512], F32, st)
        spr = Ring(S, 'SPf', 7, [128, 512], BF16, st)
        t2r = Ring(S, 't2', 3, [128, 512], F32, st)
        wr = Ring(S, 'Wt', 5, [128, 512], BF16, st)
        sqr = Ring(S, 'sqa', 2, [128, 512], BF16, st)
        rrr = Ring(S, 'rra', 2, [128, 512], F32, st)
        ynr = Ring(S, 'yn', 2, [128, 512], BF16, st)
        CUM = [P['ps'][0], P['ps'][1]]
        OUT = P['ps'][2]
        zring = [P['ps'][3], P['ps'][4], P['ps'][5]]
        zi = [0]
        strict_b = P['strict_b']
        cumU = P['cumU_b']
        cumL = P['cumL_b']

        units = []
        for g in range(ng):
            for pr in range(4):
                kbs = list(range(4 * g + 3, -1, -1))
                for ki, kb in enumerate(kbs):
                    for hh in range(2):
                        units.append(dict(g=g, pr=pr, kb=kb, hh=hh, first=(ki == 0), last=(kb == 0),
                                          newq=(ki == 0 and hh == 0)))
        qcur = {}

        def stageA1(u):
            g, pr, kb, hh = u['g'], u['pr'], u['kb'], u['hh']
            if u['newq']:
                qt, qk = qr.next()
                for h2_ in range(2):
                    S.dma('sp', qt[h2_ * 64:(h2_ + 1) * 64, h2_, :],
                          T['qsbT'][pr * 128 + h2_ * 64:pr * 128 + (h2_ + 1) * 64, g * 512:(g + 1) * 512],
                          reads=[qk], writes=[qk], semkey=(qk, h2_))
                qcur[(g, pr)] = (qt, qk)
            qt, qk = qcur[(g, pr)]
            j = kb - 4 * g
            c0 = j * 128 if j >= 0 else 0
            u['c0'] = c0
            u['diag'] = j >= 0
            pz, pzk = zring[zi[0] % 3]
            zi[0] += 1
            S.op('pe', lambda e: e.matmul(pz[:, c0:512], lhsT=kT[:, pr, kb * 128:(kb + 1) * 128], rhs=qt[:, hh, c0:512],
                                          start=True, stop=True),
                 reads=[('kT', pr), qk], writes=[pzk])
            et, ek = er.next()
            S.op('act', lambda e: e.activation(out=et[:, c0:512], in_=pz[:, c0:512], func=AF.Exp, scale=-1.0),
                 reads=[pzk], writes=[ek])
            S.op('act', lambda e: e.activation(out=et[:, c0:512], in_=et[:, c0:512], func=AF.Ln, bias=1.0),
                 reads=[ek], writes=[ek])
            u['et'] = (et, ek)
            u['pz'] = (pz, pzk)

        def stageA2(u):
            c0 = u['c0']
            et, ek = u['et']
            pz, pzk = u['pz']
            sp, spk = spr.next()
            S.op('dve', lambda e: e.tensor_tensor(out=sp[:, c0:512], in0=pz[:, c0:512], in1=et[:, c0:512], op=ALU.add),
                 reads=[pzk, ek], writes=[spk])
            if u['diag']:
                S.op('pool', lambda e: e.tensor_tensor(out=sp[:, c0:c0 + 128], in0=sp[:, c0:c0 + 128],
                                                       in1=strict_b[:, :], op=ALU.mult),
                     reads=[spk, 'strict_b'], writes=[spk])
            u['sp'] = (sp, spk)

        def stageB1(u):
            c0, hh = u['c0'], u['hh']
            pc, pck = CUM[hh]
            sp, spk = u['sp']
            et, ek = u['et']
            S.op('pe', lambda e: e.matmul(pc[:, c0:512], lhsT=cumU[:, :], rhs=sp[:, c0:512], start=u['first'],
                                          stop=False, skip_group_check=True),
                 reads=[spk, 'cumU_b'], writes=[pck])
            t2, t2k = t2r.next()
            S.op('dve', lambda e: e.tensor_tensor(out=t2[:, c0:512], in0=pc[:, c0:512], in1=et[:, c0:512],
                                                  op=ALU.subtract),
                 reads=[pck, ek], writes=[t2k])
            u['t2'] = (t2, t2k)

        def stageB2(u):
            c0 = u['c0']
            t2, t2k = u['t2']
            wt, wk = wr.next()
            S.op('act', lambda e: e.activation(out=wt[:, c0:512], in_=t2[:, c0:512], func=AF.Exp),
                 reads=[t2k], writes=[wk])
            if u['diag']:
                S.op('pool', lambda e: e.tensor_tensor(out=wt[:, c0:c0 + 128], in0=wt[:, c0:c0 + 128],
                                                       in1=strict_b[:, :], op=ALU.mult),
                     reads=[wk, 'strict_b'], writes=[wk])
            u['wt'] = (wt, wk)

        def stageC(u, which):
            c0, hh, pr, kb, g = u['c0'], u['hh'], u['pr'], u['kb'], u['g']
            pc, pck = CUM[hh]
            sp, spk = u['sp']
            wt, wk = u['wt']
            po, pok = OUT
            if which == 1:
                if not u['last']:
                    S.op('pe', lambda e: e.matmul(pc[:, c0:512], lhsT=cumL[:, :], rhs=sp[:, c0:512], start=False,
                                                  stop=False, skip_group_check=True),
                         reads=[spk, 'cumL_b'], writes=[pck])
                return
            hd = 2 * pr + hh
            S.op('pe', lambda e: e.matmul(po[:, c0:512], lhsT=Vs[:, kb, hd * 128:(hd + 1) * 128],
                                          rhs=wt[:, c0:512], start=(u['first'] and hh == 0),
                                          stop=(u['last'] and hh == 1), skip_group_check=True),
                 reads=[wk, ('Vs', kb // 8)], writes=[pok])
            if u['last'] and hh == 1:
                yn, ynk = ynr.next()
                _head_norm_T(S, P, po, pok, gsb[:, pr:pr + 1], 'gsb', yn, ynk, rrr, sqr)
                S.dma('sp', T['mixedT'][pr * 128:(pr + 1) * 128, g * 512:(g + 1) * 512], yn[:, :], reads=[ynk],
                      semkey=ynk)

        saved = P['ps'], P['psi']
        P['ps'] = [saved[0][6]]
        P['psi'] = 0
        n = len(units)
        S.barrier_exclude = {('dma', 'uvconv')}
        nconv = NEXP // 128
        conv_done = [0]
        for i in range(n + 4):
            want = min(nconv, (i * nconv) // max(1, n - 8) + 1)
            while conv_done[0] < want:
                c = conv_done[0]
                S.dma('pool', T['uvb'][c * 128:(c + 1) * 128, :], T['expert_uv'][c * 128:(c + 1) * 128, :],
                      semkey='uvconv')
                conv_done[0] += 1
            if i < n:
                stageA1(units[i])
            if 0 <= i - 1 < n:
                stageA2(units[i - 1])
            if 0 <= i - 4 < n:
                stageC(units[i - 4], 1)
                stageC(units[i - 4], 2)
            if 0 <= i - 2 < n:
                stageB1(units[i - 2])
            if 0 <= i - 3 < n:
                stageB2(units[i - 3])
            if other is not None and i % every == 0:
                next(other, None)
        if other is not None:
            for _ in other:
                pass
        P['ps'] = [saved[0][i] for i in range(7)]
        P['psi'] = saved[1]
        _barrier(S)


def phase_B3(S, T, P, Slen):
    nt = Slen // 128
    identb = P['ident_b']
    psb = P['psb']
    tri = P['tri']
    ones = P['ones']
    LNS = float(np.log(128.0 ** -0.5))
    bk = P['ps_all']
    bA, kA = bk[0]
    bS, kS = bk[1]
    bN = [bk[2], bk[3]]
    bP = [bk[4], bk[5]]
    bX, kX = bk[6]
    with ExitStack() as st:
        tris = S.sb('tris', [128, 128], F32, st)
        S.op('dve', lambda e: e.tensor_scalar(out=tris[:, :], in0=tri[:, :], scalar1=128.0 ** -0.5, scalar2=None,
                                              op0=ALU.mult), reads=['tri'], writes=['tris'])
        gml = S.sb('gml', [128, 512], F32, st)
        S.dma('sp', gml[:, :], T['gml_bc'][:, :], writes=['gml'], semkey='gml')
        C = S.sb('Cst', [128, 4, 129], F32, st)
        Cb = S.sb('Cbf', [128, 4, 129], BF16, st)
        S.op('dve', lambda e: e.memset(C[:, :, :], 0.0), writes=[('C', h) for h in range(4)])
        S.op('dve', lambda e: e.memset(Cb[:, :, :], 0.0), writes=[('Cb', h) for h in range(4)])
        qr = Ring(S, 'qml', 4, [128, 4, 128], BF16, st)
        kr = Ring(S, 'kml', 4, [128, 4, 128], BF16, st)
        vr = Ring(S, 'vaug', 4, [128, 4, 129], BF16, st)
        for t, k in vr.tiles:
            S.op('dve', lambda e, t=t: e.memset(t[:, :, 128:129], 1.0), writes=[k])
        gr = Ring(S, 'gtm', 4, [128, 8], F32, st)
        sor = Ring(S, 'sgo', 4, [128, 512], BF16, st)
        bir = Ring(S, 'bias', 2, [128, 4], F32, st)
        ebr = Ring(S, 'ebias', 2, [128, 4], F32, st)
        lfr = Ring(S, 'lfbc', 4, [128, 128], F32, st)
        eqr = Ring(S, 'EQ', 2, [128, 4, 128], F32, st)
        decr = Ring(S, 'dec', 2, [128, 4], F32, st)
        dtr = Ring(S, 'DT', 2, [128, 4, 128], F32, st)
        dmr = Ring(S, 'DM', 2, [128, 4, 128], F32, st)
        smr = Ring(S, 'SM', 2, [128, 4, 128], BF16, st)
        qgr = Ring(S, 'qg', 2, [128, 4, 128], BF16, st)
        ktr = Ring(S, 'ktok', 2, [128, 4, 128], BF16, st)
        dnr = Ring(S, 'dn', 8, [128, 2], F32, st)
        tcr = Ring(S, 'tmpC', 2, [128, 129], F32, st)
        hmlr = Ring(S, 'hml', 2, [128, 512], F32, st)
        ss4r = Ring(S, 'ss4', 2, [128, 4], F32, st)
        jr = Ring(S, 'junk3', 2, [128, 128], BF16, st)
        ytr = Ring(S, 'yt', 2, [128, 512], F32, st)
        ymr = Ring(S, 'ym', 2, [128, 512], BF16, st)
        ystr = Ring(S, 'yst', 2, [128, 4, 128], BF16, st)
        qv = T['qmlT'].ap().rearrange("(h d) t -> d h t", d=128)
        kv = T['kmlT'].ap().rearrange("(h d) t -> d h t", d=128)
        mv = T['mixedT'].ap()[512:1024, :].rearrange("(h p) t -> p h t", p=128)
        state = {}
        epi_state = {}

        loaded = {}

        def pre_load(i):
            ts_ = slice(i * 128, (i + 1) * 128)
            qt, qk = qr.next()
            kt, kk = kr.next()
            vt, vk = vr.next()
            gt, gk = gr.next()
            so, sok = sor.next()
            S.dma('sp', qt[:, :, :], qv[:, :, ts_], writes=[qk], semkey=qk)
            S.dma('sp', kt[:, :, :], kv[:, :, ts_], writes=[kk], semkey=kk)
            S.dma('sp', vt[:, :, 0:128], T['vml'].ap()[ts_, :].rearrange("p (h d) -> p h d", h=4), writes=[vk], semkey=vk)
            S.dma('sp', gt[:, :], T['gates'][ts_, :], writes=[gk], semkey=gk)
            S.dma('sp', so[:, :], T['sigo'][ts_, :], writes=[sok], semkey=sok)
            loaded[i] = (qt, qk, kt, kk, vt, vk, gt, gk, so, sok)

        def pre(i):
            qt, qk, kt, kk, vt, vk, gt, gk, so, sok = loaded.pop(i)
            S.op('pe', lambda e: e.matmul(bX[:, 0:4], lhsT=tri[:, :], rhs=gt[:, 4:8], start=True, stop=True),
                 reads=['tri', gk], writes=[kX])
            bi, bik = bir.next()
            S.op('dve', lambda e: e.tensor_tensor(out=bi[:, :], in0=gt[:, 0:4], in1=bX[:, 0:4], op=ALU.subtract),
                 reads=[gk, kX], writes=[bik])
            eb, ebk = ebr.next()
            S.op('act', lambda e: e.activation(out=eb[:, :], in_=bi[:, :], func=AF.Exp), reads=[bik], writes=[ebk])
            for hd in range(4):
                lf, lfk = lfr.next()
                S.op('dve', lambda e, hd=hd, lf=lf: e.tensor_scalar(out=lf[:, :], in0=ones[:, :],
                                                                    scalar1=gt[:, 4 + hd:5 + hd], scalar2=None,
                                                                    op0=ALU.mult), reads=['ones', gk], writes=[lfk])
                S.op('pe', lambda e, hd=hd, lf=lf: e.matmul(bA[:, hd * 128:(hd + 1) * 128], lhsT=lf[:, :], rhs=tri[:, :],
                                                            start=True, stop=True, skip_group_check=True),
                     reads=[lfk, 'tri'], writes=[kA])
            for hd in range(4):
                S.op('pe', lambda e, hd=hd: e.matmul(bS[:, hd * 128:(hd + 1) * 128], lhsT=kt[:, hd, :], rhs=qt[:, hd, :],
                                                     start=True, stop=True, skip_group_check=True),
                     reads=[kk, qk], writes=[kS])
            for hd in range(4):
                S.op('pe', lambda e, hd=hd: e.transpose(out=psb[:, hd * 128:(hd + 1) * 128], in_=kt[:, hd, :],
                                                        identity=identb[:, :]), reads=[kk, 'ident_b'], writes=['psb'])
            eq, eqk = eqr.next()
            S.op('act', lambda e: e.activation(out=eq[:, :, :], in_=bA[:, :].rearrange("p (h t) -> p h t", h=4),
                                               func=AF.Exp, bias=LNS), reads=[kA], writes=[eqk])
            dec, deck = decr.next()
            S.op('act', lambda e: e.activation(out=dec[:, :], in_=bA[:, :].rearrange("p (h t) -> p h t", h=4)[:, :, 127],
                                               func=AF.Exp), reads=[kA], writes=[deck])
            dt_, dtk = dtr.next()
            for hd in range(4):
                S.op('act', lambda e, hd=hd: e.activation(out=dt_[:, hd, :], in_=bA[:, hd * 128:(hd + 1) * 128],
                                                          func=AF.Exp, bias=bi[:, hd:hd + 1]),
                     reads=[kA, bik], writes=[dtk])
            ktk_t, ktk = ktr.next()
            for hd in range(4):
                S.op('act', lambda e, hd=hd: e.activation(out=ktk_t[:, hd, :], in_=psb[:, hd * 128:(hd + 1) * 128],
                                                          func=AF.Copy, scale=eb[:, hd:hd + 1]),
                     reads=['psb', ebk], writes=[ktk])
            dm, dmk = dmr.next()
            S.op('pool', lambda e: e.tensor_tensor(out=dm[:, :, :], in0=dt_[:, :, :],
                                                   in1=tris[:, :].unsqueeze(1).to_broadcast([128, 4, 128]), op=ALU.mult),
                 reads=[dtk, 'tris'], writes=[dmk])
            qg, qgk = qgr.next()
            S.op('pool', lambda e: e.tensor_tensor(out=qg[:, :, :], in0=qt[:, :, :], in1=eq[:, :, :], op=ALU.mult),
                 reads=[qk, eqk], writes=[qgk])
            sm, smk = smr.next()
            S.op('dve', lambda e: e.tensor_tensor(out=sm[:, :, :], in0=bS[:, :].rearrange("p (h t) -> p h t", h=4),
                                                  in1=dm[:, :, :], op=ALU.mult), reads=[kS, dmk], writes=[smk])
            state[i] = dict(vt=vt, vk=vk, so=so, sok=sok, dec=dec, deck=deck, sm=sm, smk=smk, qg=qg, qgk=qgk,
                            ktk_t=ktk_t, ktk=ktk)

        def post_pe(i):
            d = state[i]
            vt, vk = d['vt'], d['vk']
            sm, smk, qg, qgk, ktk_t, ktk = d['sm'], d['smk'], d['qg'], d['qgk'], d['ktk_t'], d['ktk']
            for hd in range(4):
                pN, pNk = bN[hd // 2]
                c0 = (hd % 2) * 256
                S.op('pe', lambda e, hd=hd, pN=pN, c0=c0: e.matmul(pN[:, c0:c0 + 129], lhsT=sm[:, hd, :], rhs=vt[:, hd, :],
                                                                   start=True, stop=False, skip_group_check=True),
                     reads=[smk, vk], writes=[pNk])
                S.op('pe', lambda e, hd=hd, pN=pN, c0=c0: e.matmul(pN[:, c0:c0 + 129], lhsT=qg[:, hd, :], rhs=Cb[:, hd, :],
                                                                   start=False, stop=True, skip_group_check=True),
                     reads=[qgk, ('Cb', hd)], writes=[pNk])
            for hd in range(4):
                pP, pPk = bP[hd // 2]
                c0 = (hd % 2) * 256
                S.op('pe', lambda e, hd=hd, pP=pP, c0=c0: e.matmul(pP[:, c0:c0 + 129], lhsT=ktk_t[:, hd, :], rhs=vt[:, hd, :],
                                                                   start=True, stop=True, skip_group_check=True),
                     reads=[ktk, vk], writes=[pPk])

        def post_dve(i):
            d = state[i]
            so, sok, dec, deck = d['so'], d['sok'], d['dec'], d['deck']
            hml, hmk = hmlr.next()
            dns = [dnr.next() for _ in range(4)]
            info = [(bN[hd // 2][0], bN[hd // 2][1], (hd % 2) * 256) for hd in range(4)]
            for hd in range(4):
                pN, pNk, c0 = info[hd]
                dn, dnk = dns[hd]
                S.op('dve', lambda e, pN=pN, c0=c0, dn=dn: e.tensor_scalar(
                    out=dn[:, 0:1], in0=pN[:, c0 + 128:c0 + 129], scalar1=-1.0, scalar2=1.0, op0=ALU.mult, op1=ALU.max),
                    reads=[pNk], writes=[(dnk, 0)])
            for hd in range(4):
                pN, pNk, c0 = info[hd]
                dn, dnk = dns[hd]
                S.op('dve', lambda e, pN=pN, c0=c0, dn=dn: e.tensor_scalar(
                    out=dn[:, 1:2], in0=pN[:, c0 + 128:c0 + 129], scalar1=1.0, scalar2=None, op0=ALU.max),
                    reads=[pNk], writes=[(dnk, 1)])
            for hd in range(4):
                dn, dnk = dns[hd]
                S.op('dve', lambda e, dn=dn: e.tensor_tensor(out=dn[:, 0:1], in0=dn[:, 0:1], in1=dn[:, 1:2], op=ALU.max),
                     reads=[(dnk, 0), (dnk, 1)], writes=[(dnk, 0)])
            for hd in range(4):
                dn, dnk = dns[hd]
                S.op('dve', lambda e, dn=dn: e.reciprocal(out=dn[:, 1:2], in_=dn[:, 0:1]), reads=[(dnk, 0)],
                     writes=[(dnk, 1)])
            for hd in range(4):
                pN, pNk, c0 = info[hd]
                dn, dnk = dns[hd]
                hs = slice(hd * 128, (hd + 1) * 128)
                S.op('dve', lambda e, pN=pN, c0=c0, dn=dn, hs=hs: e.tensor_scalar(
                    out=hml[:, hs], in0=pN[:, c0:c0 + 128], scalar1=dn[:, 1:2], scalar2=None, op0=ALU.mult),
                    reads=[pNk, (dnk, 1)], writes=[(hmk, hd)])
            for hd in range(4):
                pP, pPk = bP[hd // 2]
                c0 = (hd % 2) * 256
                tc_, tck = tcr.next()
                S.op('dve', lambda e, hd=hd, pP=pP, c0=c0, tc_=tc_: e.tensor_tensor(
                    out=tc_[:, :], in0=pP[:, c0:c0 + 129], in1=C[:, hd, :], op=ALU.add),
                    reads=[pPk, ('C', hd)], writes=[tck])
                S.op('dve', lambda e, hd=hd, tc_=tc_: e.tensor_scalar(out=C[:, hd, :], in0=tc_[:, :],
                                                                      scalar1=dec[:, hd:hd + 1], scalar2=None,
                                                                      op0=ALU.mult), reads=[tck, deck], writes=[('C', hd)])
            epi_state[i] = dict(hml=hml, hmk=hmk, so=so, sok=sok)
            state.pop(i)

        def cb(i):
            for hd in range(4):
                S.op('act', lambda e, hd=hd: e.activation(out=Cb[:, hd, :], in_=C[:, hd, :], func=AF.Copy),
                     reads=[('C', hd)], writes=[('Cb', hd)])

        def epi(i):
            ts_ = slice(i * 128, (i + 1) * 128)
            d = epi_state.pop(i)
            hml, hmk, so, sok = d['hml'], d['hmk'], d['so'], d['sok']
            ss4, s4k = ss4r.next()
            for hd in range(4):
                hs = slice(hd * 128, (hd + 1) * 128)
                jk_t, jk = jr.next()
                S.op('act', lambda e, hd=hd, hs=hs, jk_t=jk_t: e.activation(
                    out=jk_t[:, :], in_=hml[:, hs], func=AF.Square, accum_out=ss4[:, hd:hd + 1]),
                    reads=[(hmk, hd)], writes=[jk, (s4k, hd)])
            S.op('act', lambda e: e.activation(out=ss4[:, :], in_=ss4[:, :], func=AF.Ln, scale=1.0 / 128, bias=EPS),
                 reads=[(s4k, h) for h in range(4)], writes=[s4k])
            S.op('act', lambda e: e.activation(out=ss4[:, :], in_=ss4[:, :], func=AF.Exp, scale=-0.5),
                 reads=[s4k], writes=[s4k])
            yt, ytk = ytr.next()
            for hd in range(4):
                hs = slice(hd * 128, (hd + 1) * 128)
                S.op('dve', lambda e, hd=hd, hs=hs: e.scalar_tensor_tensor(
                    out=yt[:, hs], in0=hml[:, hs], scalar=ss4[:, hd:hd + 1], in1=gml[:, hs], op0=ALU.mult, op1=ALU.mult),
                    reads=[(hmk, hd), s4k, 'gml'], writes=[(ytk, hd)])
            ym, ymk = ymr.next()
            S.op('dve', lambda e: e.tensor_tensor(out=ym[:, :], in0=yt[:, :], in1=so[:, :], op=ALU.mult),
                 reads=[(ytk, h) for h in range(4)] + [sok], writes=[ymk])
            epi_state[i] = dict(ym=ym, ymk=ymk)

        def epi_b(i):
            ts_ = slice(i * 128, (i + 1) * 128)
            d = epi_state.pop(i)
            ym, ymk = d['ym'], d['ymk']
            for hd in range(4):
                S.op('pe', lambda e, hd=hd: e.transpose(out=psb[:, 512 + hd * 128:512 + (hd + 1) * 128],
                                                        in_=ym[:, hd * 128:(hd + 1) * 128], identity=identb[:, :]),
                     reads=[ymk, 'ident_b'], writes=['psb'])
            ys, ysk = ystr.next()
            S.op('act', lambda e: e.activation(out=ys[:, :, :], in_=psb[:, 512:1024].rearrange("p (h t) -> p h t", h=4),
                                               func=AF.Copy), reads=['psb'], writes=[ysk])
            S.dma('sp', mv[:, :, ts_], ys[:, :, :], reads=[ysk], semkey=ysk)

        for j in range(min(3, nt)):
            pre_load(j)
        pre(0)
        for i in range(nt + 1):
            if i >= 1:
                epi(i - 1)
            if i < nt:
                post_pe(i)
                post_dve(i)
            if i >= 1:
                epi_b(i - 1)
            if i + 1 < nt:
                pre(i + 1)
            if i < nt:
                cb(i)
            if i + 3 < nt:
                pre_load(i + 3)
        _barrier(S)


def phase_B4(S, T, P, Slen):
    mod = P['mod']
    ng = Slen // 512
    with ExitStack() as st:
        w_out = S.sb('w_out', [128, 8, D], BF16, st)
        wov = T['w_out'].ap().rearrange("(j p) n -> p j n", p=128)
        WO = []
        for j in range(8):
            WO.append(('w_out', j))
            S.dma('pool', w_out[:, j, :], wov[:, j, :], writes=[('w_out', j)], semkey='w_out')
        mr = Ring(S, 'mT', 2, [128, 8, 512], BF16, st)
        xr = Ring(S, 'xo', 3, [128, D], F32, st)
        tr = Ring(S, 'to', 2, [128, D], F32, st)
        mview = T['mixedT'].ap().rearrange("(j p) t -> p j t", p=128)
        for g in range(ng):
            mt, mk = mr.next()
            S.dma('sp', mt[:, :, :], mview[:, :, g * 512:(g + 1) * 512], writes=[mk], semkey=mk)
            for tl in range(4):
                t0 = g * 512 + tl * 128
                xt, xk = xr.next()
                S.dma('sp', xt[:, :], T['x'][t0:t0 + 128, :], writes=[xk], semkey=xk)
                tt, tk = tr.next()
                for half in range(2):
                    pst, pk = _psnext(P)
                    cs = slice(half * 512, (half + 1) * 512)
                    for j in range(8):
                        S.op('pe', lambda e, j=j: e.matmul(pst[:, :], lhsT=mt[:, j, tl * 128:(tl + 1) * 128],
                                                           rhs=w_out[:, j, cs], start=(j == 0), stop=(j == 7)),
                             reads=WO + [mk], writes=[pk])
                    S.op('dve', lambda e: e.tensor_tensor(out=tt[:, cs], in0=pst[:, :], in1=mod[:, 2048 + half * 512:2048 + (half + 1) * 512],
                                                          op=ALU.mult), reads=[pk, 'mod'], writes=[tk])
                S.op('dve', lambda e: e.tensor_tensor(out=xt[:, :], in0=tt[:, :], in1=xt[:, :], op=ALU.add),
                     reads=[tk, xk], writes=[xk])
                S.dma('sp', T['x1'][t0:t0 + 128, :], xt[:, :], reads=[xk], semkey=('x1o', xk))
        _barrier(S)


def phase_C(S, T, P, Slen):
    mod = P['mod']
    nt = Slen // 128
    identb = P['ident_b']
    psb = P['psb']
    S.barrier_exclude = ()
    _barrier(S)
    KB = 2
    with ExitStack() as st:
        w_q, WQ, keys, KEYS = P['w_q'], P['WQ'], P['keys'], P['KEYS']
        iota_i = S.sb('iota_i', [128, 16], I32, st)
        iota_f = S.sb('iota_f', [128, 16], F32, st)
        S.op('pool', lambda e: e.iota(iota_i[:, :], pattern=[[1, 16]], base=0, channel_multiplier=0),
             writes=['iota_i'])
        S.op('dve', lambda e: e.tensor_copy(out=iota_f[:, :], in_=iota_i[:, :]), reads=['iota_i'], writes=['iota_f'])
        xr = Ring(S, 'x1t', 2, [128, D], F32, st)
        h2r = Ring(S, 'h2', 1, [128, D], F32, st)
        junk = S.sb('junkc', [128, D], BF16, st)
        ssr = Ring(S, 'ssc', 2, [128, 1], F32, st)
        hbr = Ring(S, 'hbc', 2, [128, D], BF16, st)
        pdr = Ring(S, 'pd', 3, [128, D], BF16, st)
        h2T = S.sb('h2T', [128, 8, 128], BF16, st)
        qTs = S.sb('qTs', [128, 16, 128], BF16, st)
        sc = S.sb('sc', [128, 16, 128], F32, st)
        scr8 = S.sb('scr8', [128, 2048], F32, st)
        sc2 = scr8[:, :].rearrange('p (a b) -> p a b', a=16)
        tv = S.sb('tv', [128, 16, 16], F32, st)
        ti = S.sb('ti', [128, 16, 16], U32, st)
        tif = S.sb('tif', [128, 16, 16], F32, st)
        cand = S.sb('cand', [128, 8, 256], F32, st)
        cand2 = scr8[:, :].rearrange('p (a b) -> p a b', a=8)
        bv = S.sb('bv', [128, 8, 16], F32, st)
        bp = S.sb('bp', [128, 8, 16], U32, st)
        au = S.sb('au', [128, 8, 16], U32, st)
        bu = S.sb('bu', [128, 8, 16], U32, st)
        af = S.sb('af', [128, 8, 16], F32, st)
        bf = S.sb('bf', [128, 8, 16], F32, st)
        oh = scr8[:, :].rearrange('p (a b c) -> p a b c', a=8, b=16)
        i1 = S.sb('i1', [128, 8, 16], F32, st)
        i2 = S.sb('i2', [128, 8, 16], F32, st)
        ef = S.sb('ef', [128, 128], F32, st)
        idxr = Ring(S, 'idx', 2, [128, 128], I32, st)
        nmax = S.sb('nmax', [128, 8], F32, st)
        gexp = S.sb('gexp', [128, 8, 16], F32, st)
        gsum = S.sb('gsum', [128, 8], F32, st)
        gater = Ring(S, 'gate', 2, [128, 8, 16], F32, st)
        prer = Ring(S, 'pre', 2, [128, 128], F32, st)
        wgr = Ring(S, 'wg', 2, [128, 128], F32, st)
        wgtr = Ring(S, 'wgt', 2, [128, 128], F32, st)
        dgr = Ring(S, 'dg', 4, [128, 128], BF16, st)
        ur = Ring(S, 'UV', 16, [128, 2 * D], BF16, st)
        yr = Ring(S, 'yacc', 1, [128, D], F32, st)
        tv4 = tv[:, :, :].rearrange("p (h two) k -> p h two k", two=2)
        tif4 = tif[:, :, :].rearrange("p (h two) k -> p h two k", two=2)
        cand4 = cand[:, :, :].rearrange("p h (a b) -> p h a b", a=16)
        saved = P['ps'], P['psi']
        allps = saved[0]
        P['ps'] = [allps[4], allps[5], allps[6]]
        P['psi'] = 0
        tile_state = {}

        def front(i):
            ts_ = slice(i * 128, (i + 1) * 128)
            xt, xk = xr.next()
            S.dma('sp', xt[:, :], T['x1'][ts_, :], writes=[xk], semkey=xk)
            yield
            yield
            ss, sk = ssr.next()
            S.op('act', lambda e: e.activation(out=junk[:, :], in_=xt[:, :], func=AF.Square, accum_out=ss[:, :]),
                 reads=[xk], writes=['junkc', sk])
            S.op('act', lambda e: e.activation(out=ss[:, :], in_=ss[:, :], func=AF.Ln, scale=1.0 / D, bias=EPS),
                 reads=[sk], writes=[sk])
            S.op('act', lambda e: e.activation(out=ss[:, :], in_=ss[:, :], func=AF.Exp, scale=-0.5),
                 reads=[sk], writes=[sk])
            yield
            h2, hk = h2r.next()
            S.op('dve', lambda e: e.scalar_tensor_tensor(out=h2[:, :], in0=xt[:, :], scalar=ss[:, 0:1],
                                                         in1=mod[:, 4096:5120], op0=ALU.mult, op1=ALU.mult),
                 reads=[xk, sk, 'mod'], writes=[hk])
            S.op('dve', lambda e: e.tensor_tensor(out=h2[:, :], in0=h2[:, :], in1=mod[:, 3072:4096], op=ALU.add),
                 reads=[hk, 'mod'], writes=[hk])
            yield
            hb, hbk = hbr.next()
            S.op('act', lambda e: e.activation(out=hb[:, :], in_=h2[:, :], func=AF.Copy), reads=[hk], writes=[hbk])
            yield
            for j in range(8):
                S.op('pe', lambda e, j=j: e.transpose(out=psb[:, j * 128:(j + 1) * 128], in_=hb[:, j * 128:(j + 1) * 128],
                                                      identity=identb[:, :]), reads=[hbk, 'ident_b'], writes=['psb'])
            yield
            S.op('act', lambda e: e.activation(out=h2T[:, :, :], in_=psb[:, :].rearrange("p (j t) -> p j t", j=8),
                                               func=AF.Copy), reads=['psb'], writes=['h2T'])
            yield
            pend = None
            for q4 in range(4):
                pst, pk = _psnext(P)
                for hpi in range(4):
                    hp = q4 * 4 + hpi
                    for j in range(8):
                        S.op('pe', lambda e, j=j, hp=hp, hpi=hpi, pst=pst: e.matmul(
                            pst[:, hpi * 128:(hpi + 1) * 128], lhsT=w_q[:, j, hp * 128:(hp + 1) * 128], rhs=h2T[:, j, :],
                            start=(j == 0), stop=(j == 7), skip_group_check=True), reads=WQ + ['h2T'], writes=[pk])
                    if hpi == 0 and pend is not None:
                        pend()
                        pend = None
                    if hpi % 2 == 1:
                        yield
                pend = (lambda pst=pst, pk=pk, q4=q4: S.op('act', lambda e: e.activation(
                    out=qTs[:, q4 * 4:(q4 + 1) * 4, :], in_=pst[:, :].rearrange("p (a t) -> p a t", a=4), func=AF.Copy),
                    reads=[pk], writes=[('qTs', q4)]))
            pend()
            yield
            pend = None
            for q4 in range(4):
                pst, pk = _psnext(P)
                for hpi in range(4):
                    hp = q4 * 4 + hpi
                    S.op('pe', lambda e, hp=hp, hpi=hpi, pst=pst: e.matmul(
                        pst[:, hpi * 128:(hpi + 1) * 128], lhsT=qTs[:, hp, :], rhs=keys[:, hp, :], start=True, stop=True,
                        skip_group_check=True), reads=[('qTs', q4)] + KEYS, writes=[pk])
                if pend is not None:
                    pend()
                pend = (lambda pst=pst, pk=pk, q4=q4: S.op('act', lambda e: e.activation(
                    out=sc[:, q4 * 4:(q4 + 1) * 4, :], in_=pst[:, :].rearrange("p (a t) -> p a t", a=4), func=AF.Copy),
                    reads=[pk], writes=[('sc', q4)]))
                yield
            pend()
            yield
            for g4 in range(4):
                hps = [g4 * 4 + q_ for q_ in range(4)]
                sck = ('sc', g4)
                for hp in hps:
                    S.op('dve', lambda e, hp=hp: e.max(out=tv[:, hp, 0:8], in_=sc[:, hp, :]), reads=[sck], writes=[('tv', hp)])
                for hp in hps:
                    S.op('dve', lambda e, hp=hp: e.max_index(out=ti[:, hp, 0:8], in_max=tv[:, hp, 0:8], in_values=sc[:, hp, :]),
                         reads=[sck, ('tv', hp)], writes=[('ti', hp)])
                for hp in hps:
                    S.op('dve', lambda e, hp=hp: e.match_replace(out=sc2[:, hp, :], in_to_replace=tv[:, hp, 0:8],
                                                                 in_values=sc[:, hp, :], imm_value=-1e30),
                         reads=[sck, ('tv', hp)], writes=[('scr8', hp)])
                yield
                for hp in hps:
                    S.op('dve', lambda e, hp=hp: e.max(out=tv[:, hp, 8:16], in_=sc2[:, hp, :]), reads=[('scr8', hp)],
                         writes=[('tv', hp)])
                for hp in hps:
                    S.op('dve', lambda e, hp=hp: e.max_index(out=ti[:, hp, 8:16], in_max=tv[:, hp, 8:16],
                                                             in_values=sc2[:, hp, :]), reads=[('scr8', hp), ('tv', hp)],
                         writes=[('ti', hp)])
                yield
            S.op('dve', lambda e: e.tensor_copy(out=tif[:, :, :], in_=ti[:, :, :]), reads=[('ti', q_) for q_ in range(16)], writes=['tif'])
            S.op('dve', lambda e: e.tensor_tensor(
                out=cand4, in0=tv4[:, :, 0:1, :].rearrange("p h o k -> p h k o").to_broadcast([128, 8, 16, 16]),
                in1=tv4[:, :, 1:2, :].to_broadcast([128, 8, 16, 16]), op=ALU.add), reads=[('tv', q_) for q_ in range(16)], writes=['cand'])
            yield
            for g4 in range(2):
                hs_ = [g4 * 4 + q_ for q_ in range(4)]
                for h in hs_:
                    S.op('dve', lambda e, h=h: e.max(out=bv[:, h, 0:8], in_=cand[:, h, :]), reads=['cand'], writes=[('bv', h)])
                for h in hs_:
                    S.op('dve', lambda e, h=h: e.max_index(out=bp[:, h, 0:8], in_max=bv[:, h, 0:8], in_values=cand[:, h, :]),
                         reads=['cand', ('bv', h)], writes=[('bp', h)])
                for h in hs_:
                    S.op('dve', lambda e, h=h: e.match_replace(out=cand2[:, h, :], in_to_replace=bv[:, h, 0:8],
                                                               in_values=cand[:, h, :], imm_value=-1e30),
                         reads=['cand', ('bv', h)], writes=[('scr8', h)])
                yield
                for h in hs_:
                    S.op('dve', lambda e, h=h: e.max(out=bv[:, h, 8:16], in_=cand2[:, h, :]), reads=[('scr8', h)],
                         writes=[('bv', h)])
                for h in hs_:
                    S.op('dve', lambda e, h=h: e.max_index(out=bp[:, h, 8:16], in_max=bv[:, h, 8:16],
                                                           in_values=cand2[:, h, :]), reads=[('scr8', h), ('bv', h)],
                         writes=[('bp', h)])
                yield
            S.op('dve', lambda e: e.tensor_scalar(out=nmax[:, :], in0=bv[:, :, 0], scalar1=-1.0, scalar2=None,
                                                  op0=ALU.mult), reads=[('bv', q_) for q_ in range(8)], writes=['nmax'])
            yield
            for h in range(8):
                S.op('act', lambda e, h=h: e.activation(out=gexp[:, h, :], in_=bv[:, h, :], func=AF.Exp,
                                                        bias=nmax[:, h:h + 1], accum_out=gsum[:, h:h + 1]),
                     reads=[('bv', q_) for q_ in range(8)] + ['nmax'], writes=['gexp', 'gsum'])
            yield
            S.op('dve', lambda e: e.reciprocal(out=gsum[:, :], in_=gsum[:, :]), reads=['gsum'], writes=['gsum'])
            gate, gak = gater.next()
            S.op('dve', lambda e: e.tensor_tensor(out=gate[:, :, :], in0=gexp[:, :, :],
                                                  in1=gsum[:, :].unsqueeze(2).to_broadcast([128, 8, 16]), op=ALU.mult),
                 reads=['gexp', 'gsum'], writes=[gak])
            yield
            S.op('dve', lambda e: e.tensor_single_scalar(out=au[:, :, :], in_=bp[:, :, :], scalar=4,
                                                         op=ALU.logical_shift_right), reads=[('bp', q_) for q_ in range(8)], writes=['au'])
            S.op('dve', lambda e: e.tensor_single_scalar(out=bu[:, :, :], in_=bp[:, :, :], scalar=15,
                                                         op=ALU.bitwise_and), reads=[('bp', q_) for q_ in range(8)], writes=['bu'])
            S.op('dve', lambda e: e.tensor_copy(out=af[:, :, :], in_=au[:, :, :]), reads=['au'], writes=['af'])
            S.op('dve', lambda e: e.tensor_copy(out=bf[:, :, :], in_=bu[:, :, :]), reads=['bu'], writes=['bf'])
            yield
            iota_b = iota_f[:, :].unsqueeze(1).unsqueeze(1).to_broadcast([128, 8, 16, 16])
            for src, half, dst, dk in ((af, 0, i1, 'i1'), (bf, 1, i2, 'i2')):
                S.op('dve', lambda e, src=src: e.tensor_tensor(
                    out=oh[:, :, :, :], in0=src[:, :, :].unsqueeze(3).to_broadcast([128, 8, 16, 16]), in1=iota_b,
                    op=ALU.is_equal), reads=['af', 'bf', 'iota_f'], writes=['scr8'] + [('scr8', q_) for q_ in range(16)])
                yield
                S.op('dve', lambda e, half=half: e.tensor_tensor(
                    out=oh[:, :, :, :], in0=oh[:, :, :, :],
                    in1=tif4[:, :, half:half + 1, :].to_broadcast([128, 8, 16, 16]), op=ALU.mult),
                    reads=['scr8', 'tif'], writes=['scr8'] + [('scr8', q_) for q_ in range(16)])
                yield
                S.op('dve', lambda e, dst=dst: e.tensor_reduce(out=dst[:, :, :], in_=oh[:, :, :, :], axis=AX.X, op=ALU.add),
                     reads=['scr8'], writes=[dk])
                yield
            S.op('dve', lambda e: e.scalar_tensor_tensor(out=ef[:, :], in0=i1[:, :, :].rearrange("p h k -> p (h k)"),
                                                         scalar=128.0, in1=i2[:, :, :].rearrange("p h k -> p (h k)"),
                                                         op0=ALU.mult, op1=ALU.add), reads=['i1', 'i2'], writes=['ef'])
            idx, idk = idxr.next()
            S.op('dve', lambda e: e.tensor_copy(out=idx[:, :], in_=ef[:, :]), reads=['ef'], writes=[idk])
            tile_state[i] = dict(xt=xt, xk=xk, h2=h2, hk=hk, idx=idx, idk=idk, gate=gate, gak=gak, hbt=hb, hbk=hbk)
            yield

        def back(i, gen):
            ts_ = slice(i * 128, (i + 1) * 128)
            stt = tile_state.pop(i)
            xt, xk, h2, hk, idx, idk, gate, gak = (stt[n] for n in ('xt', 'xk', 'h2', 'hk', 'idx', 'idk', 'gate', 'gak'))
            hbt, hbk = stt['hbt'], stt['hbk']
            gflat = gate[:, :, :].rearrange("p h k -> p (h k)")
            pre, prk = prer.next()
            wg, wgk = wgr.next()
            wgt, wtk = wgtr.next()
            yb = [allps[(i % 2) * 2], allps[(i % 2) * 2 + 1]]
            batch = {}

            def stage1(k0):
                slots = []
                for k in range(k0, k0 + KB):
                    ut, uk = ur.next()
                    slots.append((ut, uk))
                    S.dma('pool', None, None, reads=[idk], writes=[uk], semkey=uk,
                          fn=lambda e, k=k, ut=ut: e.indirect_dma_start(
                              out=ut[:, :], out_offset=None, in_=T['uvb'][:, :],
                              in_offset=bass.IndirectOffsetOnAxis(ap=idx[:, k:k + 1], axis=0)))
                    pd, pdk = pdr.next()
                    S.op('dve', lambda e, ut=ut, pd=pd: e.tensor_tensor(out=pd[:, :], in0=ut[:, 0:D], in1=hbt[:, :],
                                                                        op=ALU.mult), reads=[uk, hbk], writes=[pdk])
                    S.op('act', lambda e, k=k, pd=pd: e.activation(out=pd[:, :], in_=pd[:, :], func=AF.Copy,
                                                                   accum_out=pre[:, k:k + 1]),
                         reads=[pdk], writes=[pdk, (prk, k, 'p')])
                ks = slice(k0, k0 + KB)
                S.op('act', lambda e: e.activation(out=wg[:, ks], in_=pre[:, ks], func=AF.Gelu),
                     reads=[(prk, k, 'p') for k in range(k0, k0 + KB)], writes=[(wgk, k0)])
                batch[k0] = slots

            def stage2(k0):
                slots = batch.pop(k0)
                ks = slice(k0, k0 + KB)
                S.op('dve', lambda e: e.tensor_tensor(out=wgt[:, ks], in0=wg[:, ks], in1=gflat[:, ks], op=ALU.mult),
                     reads=[(wgk, k0), gak], writes=[(wtk, k0)])
                for k in range(k0, k0 + KB):
                    ut, uk = slots[k - k0]
                    dg, dgk = dgr.next()
                    S.op('dve', lambda e, k=k, dg=dg: e.tensor_scalar(out=dg[:, :], in0=identb[:, :],
                                                                      scalar1=wgt[:, k:k + 1], scalar2=None, op0=ALU.mult),
                         reads=['ident_b', (wtk, k0)], writes=[dgk])
                    for half in range(2):
                        S.op('pe', lambda e, k=k, dg=dg, ut=ut, half=half: e.matmul(
                            yb[half][0][:, :], lhsT=dg[:, :], rhs=ut[:, D + half * 512:D + (half + 1) * 512],
                            start=(k == 0), stop=(k == 127)), reads=[dgk, uk], writes=[yb[half][1]])

            SKEW = 2
            for b_ in range(SKEW):
                stage1(b_ * KB)
            for k0 in range(0, 128, KB):
                if k0 + SKEW * KB < 128:
                    stage1(k0 + SKEW * KB)
                stage2(k0)
                if gen is not None:
                    if KB >= 2 or (k0 % 2 == 0):
                        for _ in range(2 if KB >= 4 else 1):
                            next(gen, None)
            if gen is not None:
                for _ in gen:
                    pass
            ya, yk = yr.next()
            for half in range(2):
                cs = slice(half * 512, (half + 1) * 512)
                S.op('dve', lambda e, half=half, cs=cs: e.tensor_tensor(
                    out=ya[:, cs], in0=yb[half][0][:, :], in1=mod[:, 5120 + half * 512:5120 + (half + 1) * 512],
                    op=ALU.mult), reads=[yb[half][1], 'mod'], writes=[yk])
            S.op('dve', lambda e: e.tensor_tensor(out=ya[:, :], in0=ya[:, :], in1=xt[:, :], op=ALU.add),
                 reads=[yk, xk], writes=[yk])
            S.dma('sp', T['out'][ts_, :], ya[:, :], reads=[yk], semkey=('outd', yk))

        for _ in front(0):
            pass
        for i in range(nt):
            gen = front(i + 1) if i + 1 < nt else None
            back(i, gen)
        P['ps'] = [allps[i] for i in range(7)]
        P['psi'] = saved[1]
        _barrier(S)


_NC_CACHE = {}


def kernel(**inputs):
    Slen = 4096
    if 'nc' not in _NC_CACHE:
        _NC_CACHE['nc'] = build(Slen, debug=False, phases="AB234C")
    nc = _NC_CACHE['nc']
    in_maps = [host_inputs(b, inputs, Slen) for b in range(8)]
    res = run_bass_kernel_spmd(nc, in_maps, core_ids=list(range(8)))
    out = np.stack([np.asarray(r["out"], dtype=np.float32) for r in res.results], axis=0)
    return out
```

```python
from contextlib import ExitStack
import numpy as np
import concourse.bass as bass
import concourse.mybir as mybir
from concourse.bass_utils import run_bass_kernel_spmd

F32 = mybir.dt.float32
BF16 = mybir.dt.bfloat16
U32 = mybir.dt.uint32
I32 = mybir.dt.int32
ALU = mybir.AluOpType
AF = mybir.ActivationFunctionType
AX = mybir.AxisListType


class Sync:
    def __init__(self, nc):
        self.nc = nc
        self.es = ExitStack()
        self.eng = {'pe': nc.tensor, 'act': nc.scalar, 'dve': nc.vector,
                    'pool': nc.gpsimd, 'sp': nc.sync}
        self.sem = {}
        self.cnt = {}
        self.seen = {k: {} for k in self.eng}
        self.lastw = {}
        self.readers = {}
        self.nsem = 0
        self.ninst = 0

    def __enter__(self):
        self.es.__enter__()
        for k in self.eng:
            self._mksem(k)
        return self

    def __exit__(self, *a):
        return self.es.__exit__(*a)

    def _mksem(self, key):
        if key not in self.sem:
            self.nsem += 1
            self.sem[key] = self.es.enter_context(self.nc.semaphore("s%d" % self.nsem))
            self.cnt[key] = 0
        return self.sem[key]

    def sb(self, name, shape, dtype, stack=None):
        self.nalloc = getattr(self, 'nalloc', 0) + 1
        return (stack or self.es).enter_context(
            self.nc.sbuf_tensor("sb%d_%s" % (self.nalloc, name), list(shape), dtype))

    def ps(self, name, shape, dtype, stack=None):
        self.nalloc = getattr(self, 'nalloc', 0) + 1
        return (stack or self.es).enter_context(
            self.nc.psum_tensor("pp%d_%s" % (self.nalloc, name), list(shape), dtype))

    def _waits(self, e, reads, writes, is_dma):
        need = {}

        def add(rec, raw):
            key, val, src = rec
            if (not is_dma) and src == e:
                if e == 'pe':
                    return
            if self.seen[e].get(key, 0) >= val:
                return
            if need.get(key, 0) < val:
                need[key] = val

        for b in reads:
            w = self.lastw.get(b)
            if w:
                add(w, True)
        for b in writes:
            w = self.lastw.get(b)
            if w:
                add(w, False)
            for key, (val, src) in self.readers.get(b, {}).items():
                add((key, val, src), False)
        for key, val in need.items():
            self.eng[e].wait_ge(self.sem[key], val)
            self.seen[e][key] = val

    def _record(self, rec, reads, writes):
        key, val, src = rec
        for b in reads:
            d = self.readers.setdefault(b, {})
            d[key] = (val, src)
        for b in writes:
            self.lastw[b] = rec
            self.readers[b] = {}

    def op(self, e, fn, reads=(), writes=()):
        self._waits(e, reads, writes, False)
        ins = fn(self.eng[e])
        self.cnt[e] += 1
        ins.then_inc(self.sem[e], 1)
        self.ninst += 1
        self._record((e, self.cnt[e], e), reads, writes)
        return ins

    def dma(self, q, out, in_, reads=(), writes=(), semkey=None, fn=None, **kw):
        assert semkey is not None
        key = ('dma', semkey)
        self._mksem(key)
        self._waits(q, reads, writes, True)
        if fn is not None:
            ins = fn(self.eng[q])
        else:
            ins = self.eng[q].dma_start(out=out, in_=in_, **kw)
        self.cnt[key] += 16
        ins.then_inc(self.sem[key], 16)
        self.ninst += 1
        self._record((key, self.cnt[key], 'dma'), reads, writes)
        return ins

    def finish(self, bufs):
        self._waits('sp', bufs, (), True)


D = 1024
NIN = 3592
EPS = 1e-6
NEXP = 16384


class Ring:
    def __init__(self, S, name, n, shape, dtype, st, psum=False):
        self.tiles = []
        for i in range(n):
            nm = "%s%d" % (name, i)
            t = S.ps(nm, shape, dtype, st) if psum else S.sb(nm, shape, dtype, st)
            self.tiles.append((t, (name, i)))
        self.i = 0

    def next(self):
        r = self.tiles[self.i % len(self.tiles)]
        self.i += 1
        return r


def _barrier(S):
    excl = getattr(S, 'barrier_exclude', ())
    for e in S.eng:
        for key, sem in S.sem.items():
            if key in excl:
                continue
            v = S.cnt[key]
            if v > 0 and S.seen[e].get(key, 0) < v and key != e:
                S.eng[e].wait_ge(sem, v)
                S.seen[e][key] = v


def _declare(nc, Slen, debug):
    T = {}

    def inp(name, shape, dt=F32):
        T[name] = nc.dram_tensor(name, list(shape), dt, kind="ExternalInput")

    def scr(name, shape, dt):
        T[name] = nc.dram_tensor(name, list(shape), dt,
                                 kind="ExternalOutput" if debug else "Internal")

    inp("x", [Slen, D]); inp("c_t", [128, 8]); inp("w_ada", [D, 6 * D]); inp("b_ada_bc", [128, 6 * D])
    inp("g1_bc", [128, D]); inp("g2_bc", [128, D]); inp("w_in", [D, NIN]); inp("bgate_bc", [128, 8])
    inp("convw", [128, 8, 4]); inp("gq_col", [128, 1]); inp("gk_col", [128, 1]); inp("gsb_col", [128, 4])
    inp("gml_bc", [128, 512]); inp("w_out", [D, D]); inp("w_q", [D, 2048]); inp("keysT", [128, 16, 128])
    inp("expert_uv", [NEXP, 2 * D])
    T["uvb"] = nc.dram_tensor("uvb", [NEXP, 2 * D], BF16, kind="Internal")
    T["out"] = nc.dram_tensor("out", [Slen, D], F32, kind="ExternalOutput")
    scr("qsbT", [512, Slen], BF16); scr("ksbT", [512, Slen], BF16); scr("vsb", [Slen, 1024], BF16)
    scr("qmlT", [512, Slen], BF16); scr("kmlT", [512, Slen], BF16); scr("vml", [Slen, 512], BF16)
    scr("sigo", [Slen, 512], BF16); scr("gates", [Slen, 8], F32); scr("mixedT", [D, Slen], BF16)
    scr("x1", [Slen, D], F32)
    if debug:
        scr("dbg_mod", [128, 6 * D], F32)
    return T


def _consts(S, P):
    nc = S.nc
    es = S.es
    ones = S.sb("ones", [128, 128], F32); P['ones'] = ones
    S.op('pool', lambda e: e.memset(ones[:, :], 1.0), writes=['ones'])
    neg = S.sb("negones", [128, 128], F32)
    S.op('pool', lambda e: e.memset(neg[:, :], -1.0), writes=['neg'])

    def sel(name, src, srck, cmp, step, cm, dt=None):
        t = S.sb(name, [128, 128], F32)
        S.op('pool', lambda e: e.affine_select(out=t[:, :], in_=src[:, :], pattern=[[step, 128]],
                                               compare_op=cmp, fill=0.0, base=0, channel_multiplier=cm),
             reads=[srck], writes=[name])
        P[name] = t
        if dt is not None:
            tb = S.sb(name + "_b", [128, 128], dt)
            S.op('dve', lambda e: e.tensor_copy(out=tb[:, :], in_=t[:, :]), reads=[name], writes=[name + "_b"])
            P[name + "_b"] = tb
        return t

    sel("ident", ones, 'ones', ALU.is_equal, 1, -1, BF16)
    sel("tri", ones, 'ones', ALU.is_ge, 1, -1)
    sel("strict", ones, 'ones', ALU.is_gt, 1, -1, BF16)
    sel("cumU", neg, 'neg', ALU.is_gt, -1, 1, BF16)
    sel("cumL", neg, 'neg', ALU.is_ge, 1, -1, BF16)
    bo = S.sb("blockones", [128, 128], BF16); P['blockones'] = bo
    S.op('pool', lambda e: e.memset(bo[:, :], 0.0), writes=['blockones'])
    S.op('pool', lambda e: e.memset(bo[0:64, 0:64], 1.0), reads=['blockones'], writes=['blockones'])
    S.op('pool', lambda e: e.memset(bo[64:128, 64:128], 1.0), reads=['blockones'], writes=['blockones'])
    P['ps'] = [(S.ps("ps%d" % i, [128, 512], F32), ('ps', i)) for i in range(7)]
    P['psb'] = S.ps("psb", [128, 1024], BF16)
    P['ps_all'] = list(P['ps'])
    P['psi'] = 0
    P['mod'] = S.sb("mod", [128, 6 * D], F32)


def _psnext(P):
    r = P['ps'][P['psi'] % len(P['ps'])]
    P['psi'] += 1
    return r


def _preload(S, T, P):
    w_q = S.sb('w_q', [128, 8, 2048], BF16)
    wv = T['w_q'].ap().rearrange("(j p) n -> p j n", p=128)
    WQ = []
    keys = S.sb('keysb', [128, 16, 128], BF16)
    KEYS = []
    P['w_q'], P['WQ'], P['keys'], P['KEYS'] = w_q, WQ, keys, KEYS

    def issue_late():
        for j in range(8):
            for hf in range(2):
                k = ('w_q', j, hf)
                WQ.append(k)
                S.dma('pool', w_q[:, j, hf * 1024:(hf + 1) * 1024], wv[:, j, hf * 1024:(hf + 1) * 1024],
                      writes=[k], semkey='w_q')
        for q in range(4):
            KEYS.append(('keys', q))
            S.dma('pool', keys[:, q * 4:(q + 1) * 4, :], T['keysT'][:, q * 4:(q + 1) * 4, :], writes=[('keys', q)],
                  semkey='keys')
    P['issue_late'] = issue_late


def _load_w_in(S, T, P, st):
    w_in = S.sb('w_in', [128, 8, NIN], BF16, st)
    w_in_v = T['w_in'].ap().rearrange("(j p) n -> p j n", p=128)
    WIN = []
    for j in range(8):
        for q in range(4):
            k = ('w_in', j, q)
            WIN.append(k)
            S.dma('pool', w_in[:, j, q * 898:(q + 1) * 898], w_in_v[:, j, q * 898:(q + 1) * 898],
                  writes=[k], semkey='w_in')
    P['w_in'], P['WIN'] = w_in, WIN


def phase_A(S, T, P):
    ones = P['ones']
    mod = P['mod']
    with ExitStack() as st:
        cT = S.sb('cT', [128, 8], F32, st)
        sc = S.sb('sc', [128, 8], F32, st)
        Lc = S.sb('Lc', [128, 8, 128], F32, st)
        wb = [S.sb('wada%d' % i, [128, 3072], F32, st) for i in range(2)]
        bada = S.sb('bada', [128, 3072], F32, st)
        gbc = S.sb('gbc', [128, 2, D], F32, st)
        S.dma('sp', cT[:, :], T['c_t'][:, :], writes=['cT'], semkey='cT')
        S.dma('sp', gbc[:, 0, :], T['g1_bc'][:, :], writes=['gbc0'], semkey='gbc0')
        S.dma('sp', gbc[:, 1, :], T['g2_bc'][:, :], writes=['gbc1'], semkey='gbc1')
        S.op('act', lambda e: e.activation(out=sc[:, :], in_=cT[:, :], func=AF.Silu), reads=['cT'], writes=['sc'])
        for j in range(8):
            S.op('dve', lambda e, j=j: e.tensor_scalar(out=Lc[:, j, :], in0=ones[:, :], scalar1=sc[:, j:j + 1],
                                                       scalar2=None, op0=ALU.mult),
                 reads=['ones', 'sc'], writes=[('Lc', j)])
        for half in range(2):
            c0 = half * 3072
            S.dma('sp', bada[:, :], T['b_ada_bc'][:, c0:c0 + 3072], writes=['bada'], semkey='bada')
            for j in range(8):
                slot = j % 2
                S.dma('sp', wb[slot][:, :], T['w_ada'][j * 128:(j + 1) * 128, c0:c0 + 3072],
                      writes=[('wb', slot)], semkey=('wb', slot))
                for n in range(6):
                    S.op('pe', lambda e, j=j, n=n, slot=slot: e.matmul(
                        P['ps'][n][0][:, :], lhsT=Lc[:, j, :], rhs=wb[slot][:, n * 512:(n + 1) * 512],
                        start=(j == 0), stop=(j == 7)),
                        reads=[('wb', slot), ('Lc', j)], writes=[('ps', n)])
            for n in range(6):
                S.op('dve', lambda e, n=n: e.tensor_tensor(
                    out=mod[:, c0 + n * 512:c0 + (n + 1) * 512], in0=P['ps'][n][0][:, :],
                    in1=bada[:, n * 512:(n + 1) * 512], op=ALU.add),
                    reads=[('ps', n), 'bada'], writes=['mod'])
        if 'dbg_mod' in T:
            S.dma('sp', T['dbg_mod'][:, :], mod[:, :], reads=['mod'], semkey='dbgmod')
        for i, c0 in ((0, 1024), (1, 4096)):
            S.op('dve', lambda e, i=i, c0=c0: e.scalar_tensor_tensor(
                out=mod[:, c0:c0 + D], in0=mod[:, c0:c0 + D], scalar=1.0, in1=gbc[:, i, :],
                op0=ALU.add, op1=ALU.mult), reads=['mod', 'gbc%d' % i], writes=['mod'])
        _barrier(S)


def phase_B1(S, T, P, Slen):
    mod = P['mod']
    ng = Slen // 512
    identb = P['ident_b']
    psb = P['psb']
    with ExitStack() as st:
        w_in, WIN = P['w_in'], P['WIN']
        cw = S.sb('cw', [128, 8, 4], F32, st)
        gq = S.sb('gq', [128, 2], F32, st)
        bg = S.sb('bg', [128, 8], F32, st)
        S.dma('sp', cw[:, :, :], T['convw'][:, :, :], writes=['cw'], semkey='cw')
        S.dma('sp', gq[:, 0:1], T['gq_col'][:, :], writes=['gq0'], semkey='gq0')
        S.dma('sp', gq[:, 1:2], T['gk_col'][:, :], writes=['gq1'], semkey='gq1')
        S.dma('sp', bg[:, :], T['bgate_bc'][:, :], writes=['bg'], semkey='bg')
        xr = Ring(S, 'xt', 2, [128, D], F32, st)
        hbr = Ring(S, 'hb', 2, [128, D], BF16, st)
        junk = S.sb('junkb', [128, D], BF16, st)
        ssr = Ring(S, 'ss', 2, [128, 1], F32, st)
        tmp32 = S.sb('tmp32', [128, D], F32, st)
        hT = S.sb('hT', [128, 8, 512], BF16, st)
        sqr = Ring(S, 'sq', 2, [128, 512], BF16, st)
        rrr = Ring(S, 'rr', 2, [128, 512], F32, st)
        qnr = Ring(S, 'qn', 2, [128, 512], BF16, st)
        cb = [S.sb('cb%d' % j, [128, 515], F32, st) for j in range(8)]
        accr = Ring(S, 'acc', 3, [128, 512], F32, st)
        sor = Ring(S, 'so', 2, [128, 512], BF16, st)
        tokr = Ring(S, 'tok', 3, [128, 512], BF16, st)
        vpr = Ring(S, 'vpad', 2, [128, 1024], BF16, st)
        for t_, k_ in vpr.tiles:
            S.op('pool', lambda e, t_=t_: e.memset(t_[:, :], 0.0), writes=[k_])
        gr = Ring(S, 'gt', 2, [128, 8], F32, st)
        etr = Ring(S, 'et', 2, [128, 4], F32, st)
        for j in range(8):
            S.op('pool', lambda e, j=j: e.memset(cb[j][:, 0:3], 0.0), writes=[('cb', j)])
        HT = [('hT', i) for i in range(4)]
        conv_tail = []
        norm_tail = []
        for g in range(ng):
            gs = slice(g * 512, (g + 1) * 512)
            for tl in range(4):
                t0 = g * 512 + tl * 128
                xt, xk = xr.next()
                S.dma('sp', xt[:, :], T['x'][t0:t0 + 128, :], writes=[xk], semkey=xk)
                ss, sk = ssr.next()
                S.op('act', lambda e: e.activation(out=junk[:, :], in_=xt[:, :], func=AF.Square, accum_out=ss[:, :]),
                     reads=[xk], writes=['junkb', sk])
                S.op('act', lambda e: e.activation(out=ss[:, :], in_=ss[:, :], func=AF.Ln, scale=1.0 / D, bias=EPS),
                     reads=[sk], writes=[sk])
                S.op('act', lambda e: e.activation(out=ss[:, :], in_=ss[:, :], func=AF.Exp, scale=-0.5),
                     reads=[sk], writes=[sk])
                S.op('dve', lambda e: e.scalar_tensor_tensor(out=tmp32[:, :], in0=xt[:, :], scalar=ss[:, 0:1],
                                                             in1=mod[:, 1024:2048], op0=ALU.mult, op1=ALU.mult),
                     reads=[xk, sk, 'mod'], writes=['tmp32'])
                hb, hk = hbr.next()
                S.op('dve', lambda e: e.tensor_tensor(out=hb[:, :], in0=tmp32[:, :], in1=mod[:, 0:1024], op=ALU.add),
                     reads=['tmp32', 'mod'], writes=[hk])
                for j in range(8):
                    S.op('pe', lambda e, j=j: e.transpose(out=psb[:, j * 128:(j + 1) * 128],
                                                          in_=hb[:, j * 128:(j + 1) * 128], identity=identb[:, :]),
                         reads=[hk, 'ident_b'], writes=['psb'])
                S.op('act', lambda e: e.activation(out=hT[:, :, tl * 128:(tl + 1) * 128],
                                                   in_=psb[:, :].rearrange("p (j t) -> p j t", j=8), func=AF.Copy),
                     reads=['psb'], writes=[('hT', tl)])
            for m in range(16):
                col0 = m * 128 if m < 8 else 1536 + (m - 8) * 128
                pst, pk = _psnext(P)
                for j in range(8):
                    S.op('pe', lambda e, j=j: e.matmul(pst[:, :], lhsT=w_in[:, j, col0:col0 + 128], rhs=hT[:, j, :],
                                                       start=(j == 0), stop=(j == 7)),
                         reads=WIN + HT, writes=[pk])
                if m < 8:
                    sq, sqk = sqr.next()
                    S.op('act', lambda e: e.activation(out=sq[:, :], in_=pst[:, :], func=AF.Square),
                         reads=[pk], writes=[sqk])
                    while norm_tail:
                        norm_tail.pop(0)()

                    def ntail(m=m, pst=pst, pk=pk, sq=sq, sqk=sqk):
                        ps2, pk2 = _psnext(P)
                        S.op('pe', lambda e: e.matmul(ps2[:, :], lhsT=P['blockones'][:, :], rhs=sq[:, :],
                                                      start=True, stop=True),
                             reads=[sqk, 'blockones'], writes=[pk2])
                        rr, rk = rrr.next()
                        S.op('act', lambda e: e.activation(out=rr[:, :], in_=ps2[:, :], func=AF.Ln, scale=1.0 / 64, bias=EPS),
                             reads=[pk2], writes=[rk])
                        S.op('act', lambda e: e.activation(out=rr[:, :], in_=rr[:, :], func=AF.Exp, scale=-0.5,
                                                           bias=(float(np.log(0.125)) if m < 4 else 0.0)),
                             reads=[rk], writes=[rk])
                        qn, qk = qnr.next()
                        gi = 0 if m < 4 else 1
                        S.op('dve', lambda e: e.scalar_tensor_tensor(out=qn[:, :], in0=pst[:, :], scalar=gq[:, gi:gi + 1],
                                                                     in1=rr[:, :], op0=ALU.mult, op1=ALU.mult),
                             reads=[pk, rk, 'gq%d' % gi], writes=[qk])
                        dst = T['qsbT'] if m < 4 else T['ksbT']
                        r0 = (m % 4) * 128
                        S.dma('sp', dst[r0:r0 + 128, gs], qn[:, :], reads=[qk], semkey=qk)
                    norm_tail.append(ntail)
                else:
                    j = m - 8
                    S.op('act', lambda e: e.activation(out=cb[j][:, 3:515], in_=pst[:, :], func=AF.Copy),
                         reads=[pk], writes=[('cb', j)])
                    while norm_tail:
                        norm_tail.pop(0)()
                    while conv_tail:
                        conv_tail.pop(0)()
                    at, ak = accr.next()
                    S.op('dve', lambda e: e.tensor_scalar(out=at[:, :], in0=cb[j][:, 3:515], scalar1=cw[:, j, 3:4],
                                                          scalar2=None, op0=ALU.mult),
                         reads=[('cb', j), 'cw'], writes=[ak])
                    for tap in (2, 1, 0):
                        S.op('dve', lambda e, tap=tap: e.scalar_tensor_tensor(
                            out=at[:, :], in0=cb[j][:, tap:tap + 512], scalar=cw[:, j, tap:tap + 1], in1=at[:, :],
                            op0=ALU.mult, op1=ALU.add), reads=[('cb', j), ak, 'cw'], writes=[ak])
                    S.op('pool', lambda e: e.tensor_copy(out=cb[j][:, 0:3], in_=cb[j][:, 512:515]),
                         reads=[('cb', j)], writes=[('cb', j)])
                    def tail(j=j, at=at, ak=ak):
                        so, sok = sor.next()
                        S.op('act', lambda e: e.activation(out=so[:, :], in_=at[:, :], func=AF.Silu),
                             reads=[ak], writes=[sok])
                        dst = T['qmlT'] if j < 4 else T['kmlT']
                        r0 = (j % 4) * 128
                        S.dma('sp', dst[r0:r0 + 128, gs], so[:, :], reads=[sok], semkey=sok)
                    conv_tail.append(tail)
            while conv_tail:
                conv_tail.pop(0)()
            for tl in range(4):
                t0 = g * 512 + tl * 128
                for c0, name in ((1024, 'vsb'), (2560, 'vml'), (3072, 'sigo')):
                    pst, pk = _psnext(P)
                    for j in range(8):
                        S.op('pe', lambda e, j=j: e.matmul(pst[:, :], lhsT=hT[:, j, tl * 128:(tl + 1) * 128],
                                                           rhs=w_in[:, j, c0:c0 + 512], start=(j == 0), stop=(j == 7)),
                             reads=WIN + [('hT', tl)], writes=[pk])
                    if name == 'vsb':
                        vp, vpk = vpr.next()
                        vp4 = vp[:, :].rearrange("p (pr two c) -> p pr two c", two=2, c=128)
                        ps4 = pst[:, :].rearrange("p (pr two d) -> p pr two d", two=2, d=64)
                        S.op('act', lambda e: e.activation(out=vp4[:, :, 0, 0:64], in_=ps4[:, :, 0, :], func=AF.Copy),
                             reads=[pk], writes=[vpk])
                        S.op('act', lambda e: e.activation(out=vp4[:, :, 1, 64:128], in_=ps4[:, :, 1, :], func=AF.Copy),
                             reads=[pk, vpk], writes=[vpk])
                        S.dma('sp', T['vsb'][t0:t0 + 128, :], vp[:, :], reads=[vpk], semkey=vpk)
                        continue
                    tt, tk = tokr.next()
                    S.op('act', lambda e: e.activation(out=tt[:, :], in_=pst[:, :],
                                                       func=(AF.Sigmoid if name == 'sigo' else AF.Copy)),
                         reads=[pk], writes=[tk])
                    S.dma('sp', T[name][t0:t0 + 128, :], tt[:, :], reads=[tk], semkey=tk)
                pst, pk = _psnext(P)
                for j in range(8):
                    S.op('pe', lambda e, j=j: e.matmul(pst[:, 0:8], lhsT=hT[:, j, tl * 128:(tl + 1) * 128],
                                                       rhs=w_in[:, j, 3584:3592], start=(j == 0), stop=(j == 7)),
                         reads=WIN + [('hT', tl)], writes=[pk])
                gt, gk = gr.next()
                S.op('dve', lambda e: e.tensor_tensor(out=gt[:, :], in0=pst[:, 0:8], in1=bg[:, :], op=ALU.add),
                     reads=[pk, 'bg'], writes=[gk])
                et, ek = etr.next()
                S.op('act', lambda e: e.activation(out=et[:, :], in_=gt[:, 4:8], func=AF.Exp, scale=-1.0),
                     reads=[gk], writes=[ek])
                S.op('act', lambda e: e.activation(out=et[:, :], in_=et[:, :], func=AF.Ln, bias=1.0),
                     reads=[ek], writes=[ek])
                S.op('dve', lambda e: e.tensor_scalar(out=gt[:, 4:8], in0=et[:, :], scalar1=-1.0, scalar2=None,
                                                      op0=ALU.mult), reads=[ek, gk], writes=[gk])
                S.dma('sp', T['gates'][t0:t0 + 128, :], gt[:, :], reads=[gk], semkey=gk)
        _barrier(S)


def build(Slen=4096, debug=False, phases="AB"):
    nc = bass.Bass("TRN2", target_bir_lowering=False)
    T = _declare(nc, Slen, debug)
    S = Sync(nc)
    P = {}
    with S:
        _consts(S, P)
        _preload(S, T, P)
        st_win = ExitStack()
        _load_w_in(S, T, P, st_win)
        P['issue_late']()
        with nc.named_scope("phA"):
            phase_A(S, T, P)
        def phase_B23(S, T, P, Slen):
            with ExitStack() as st3:
                phase_B2(S, T, P, Slen)
                _barrier(S)

        plan = [("B", phase_B1)]
        plan += [("2", phase_B2), ("3", phase_B3)]
        plan += [("4", phase_B4), ("C", phase_C)]
        for tag, fnp in plan:
            if all(t in phases for t in tag):
                with nc.named_scope("ph" + tag):
                    fnp(S, T, P, Slen)
            if tag == "B":
                st_win.close()
        _barrier(S)
        S.eng['sp'].wait_ge(S.sem['sp'], S.cnt['sp']) if S.cnt['sp'] else None
    return nc


def host_inputs(b, inputs, Slen=4096):
    f = lambda a: np.ascontiguousarray(a, dtype=np.float32)
    m = {}
    m["x"] = f(inputs["x"][b, :Slen])
    m["c_t"] = f(inputs["c"][b].reshape(8, 128).T)
    m["w_ada"] = f(inputs["w_ada"][0])
    m["b_ada_bc"] = f(np.broadcast_to(inputs["b_ada"][0][None, :], (128, 6 * D)))
    m["g1_bc"] = f(np.broadcast_to(inputs["g_norm1"][0][None, :], (128, D)))
    m["g2_bc"] = f(np.broadcast_to(inputs["g_norm2"][0][None, :], (128, D)))
    m["w_in"] = f(inputs["w_in"][0])
    bgate = np.concatenate([inputs["b_igate"][0], inputs["b_fgate"][0]])
    m["bgate_bc"] = f(np.broadcast_to(bgate[None, :], (128, 8)))
    m["convw"] = f(inputs["conv_w"][0].reshape(4, 8, 128).transpose(2, 1, 0))
    m["gq_col"] = f(np.tile(inputs["g_q_sb"][0], 2).reshape(128, 1))
    m["gk_col"] = f(np.tile(inputs["g_k_sb"][0], 2).reshape(128, 1))
    m["gsb_col"] = f(inputs["g_out_sb"][0].reshape(4, 128).T)
    m["gml_bc"] = f(np.broadcast_to(inputs["g_out_ml"][0].reshape(1, 512), (128, 512)))
    m["w_out"] = f(inputs["w_out"][0])
    m["w_q"] = f(inputs["w_q_peer"][0])
    m["keysT"] = f(inputs["sub_keys"][0].reshape(16, 128, 128).transpose(2, 0, 1))
    m["expert_uv"] = np.ascontiguousarray(
        np.concatenate([inputs["expert_u"][0], inputs["expert_v"][0]], axis=1), dtype=np.float32)
    return m


def _head_norm_T(S, P, pst, pk, gcol, gkey, out_tile, out_key, rrr, sqr, extra_bias=0.0):
    sq, sqk = sqr.next()
    S.op('act', lambda e: e.activation(out=sq[:, :], in_=pst[:, :], func=AF.Square), reads=[pk], writes=[sqk])
    ps2, pk2 = _psnext(P)
    S.op('pe', lambda e: e.matmul(ps2[:, :], lhsT=P['blockones'][:, :], rhs=sq[:, :], start=True, stop=True),
         reads=[sqk, 'blockones'], writes=[pk2])
    rr, rk = rrr.next()
    S.op('act', lambda e: e.activation(out=rr[:, :], in_=ps2[:, :], func=AF.Ln, scale=1.0 / 64, bias=EPS),
         reads=[pk2], writes=[rk])
    S.op('act', lambda e: e.activation(out=rr[:, :], in_=rr[:, :], func=AF.Exp, scale=-0.5, bias=extra_bias),
         reads=[rk], writes=[rk])
    S.op('dve', lambda e: e.scalar_tensor_tensor(out=out_tile[:, :], in0=pst[:, :], scalar=gcol, in1=rr[:, :],
                                                 op0=ALU.mult, op1=ALU.mult),
         reads=[pk, rk, gkey], writes=[out_key])


def phase_B2(S, T, P, Slen, other=None, every=7):
    ng = Slen // 512
    nt = Slen // 128
    with ExitStack() as st:
        kT = S.sb('kT', [128, 4, Slen], BF16, st)
        Vs = S.sb('Vs', [128, nt, 1024], BF16, st)
        gsb = S.sb('gsbc', [128, 4], F32, st)
        S.dma('sp', gsb[:, :], T['gsb_col'][:, :], writes=['gsb'], semkey='gsb')
        for pr in range(4):
            S.dma('sp', kT[:, pr, :], T['ksbT'][pr * 128:(pr + 1) * 128, :], writes=[('kT', pr)], semkey=('kT', pr))
        vview = T['vsb'].ap().rearrange("(n p) c -> p n c", p=128)
        for n0 in range(0, nt, 8):
            n1 = min(nt, n0 + 8)
            S.dma('sp', Vs[:, n0:n1, :], vview[:, n0:n1, :], writes=[('Vs', n0 // 8)], semkey=('Vs', n0 // 8))
        qr = Ring(S, 'qTg', 2, [128, 2, 512], BF16, st)
        for t_, k_ in qr.tiles:
            S.op('pool', lambda e, t_=t_: e.memset(t_[:, :, :], 0.0), writes=[k_])
        er = Ring(S, 'Esp', 6, [128, 512], F32, st)
        spr = Ring(S, 'SPf', 7, [128, 512], BF16, st)
        t2r = Ring(S, 't2', 3, [128, 512], F32, st)
        wr = Ring(S, 'Wt', 5, [128, 512], BF16, st)
        sqr = Ring(S, 'sqa', 2, [128, 512], BF16, st)
        rrr = Ring(S, 'rra', 2, [128, 512], F32, st)
        ynr = Ring(S, 'yn', 2, [128, 512], BF16, st)
        CUM = [P['ps'][0], P['ps'][1]]
        OUT = P['ps'][2]
        zring = [P['ps'][3], P['ps'][4], P['ps'][5]]
        zi = [0]
        strict_b = P['strict_b']
        cumU = P['cumU_b']
        cumL = P['cumL_b']

        units = []
        for g in range(ng):
            for pr in range(4):
                kbs = list(range(4 * g + 3, -1, -1))
                for ki, kb in enumerate(kbs):
                    for hh in range(2):
                        units.append(dict(g=g, pr=pr, kb=kb, hh=hh, first=(ki == 0), last=(kb == 0),
                                          newq=(ki == 0 and hh == 0)))
        qcur = {}

        def stageA1(u):
            g, pr, kb, hh = u['g'], u['pr'], u['kb'], u['hh']
            if u['newq']:
                qt, qk = qr.next()
                for h2_ in range(2):
                    S.dma('sp', qt[h2_ * 64:(h2_ + 1) * 64, h2_, :],
                          T['qsbT'][pr * 128 + h2_ * 64:pr * 128 + (h2_ + 1) * 64, g * 512:(g + 1) * 512],
                          reads=[qk], writes=[qk], semkey=(qk, h2_))
                qcur[(g, pr)] = (qt, qk)
            qt, qk = qcur[(g, pr)]
            j = kb - 4 * g
            c0 = j * 128 if j >= 0 else 0
            u['c0'] = c0
            u['diag'] = j >= 0
            pz, pzk = zring[zi[0] % 3]
            zi[0] += 1
            S.op('pe', lambda e: e.matmul(pz[:, c0:512], lhsT=kT[:, pr, kb * 128:(kb + 1) * 128], rhs=qt[:, hh, c0:512],
                                          start=True, stop=True),
                 reads=[('kT', pr), qk], writes=[pzk])
            et, ek = er.next()
            S.op('act', lambda e: e.activation(out=et[:, c0:512], in_=pz[:, c0:512], func=AF.Exp, scale=-1.0),
                 reads=[pzk], writes=[ek])
            S.op('act', lambda e: e.activation(out=et[:, c0:512], in_=et[:, c0:512], func=AF.Ln, bias=1.0),
                 reads=[ek], writes=[ek])
            u['et'] = (et, ek)
            u['pz'] = (pz, pzk)

        def stageA2(u):
            c0 = u['c0']
            et, ek = u['et']
            pz, pzk = u['pz']
            sp, spk = spr.next()
            S.op('dve', lambda e: e.tensor_tensor(out=sp[:, c0:512], in0=pz[:, c0:512], in1=et[:, c0:512], op=ALU.add),
                 reads=[pzk, ek], writes=[spk])
            if u['diag']:
                S.op('pool', lambda e: e.tensor_tensor(out=sp[:, c0:c0 + 128], in0=sp[:, c0:c0 + 128],
                                                       in1=strict_b[:, :], op=ALU.mult),
                     reads=[spk, 'strict_b'], writes=[spk])
            u['sp'] = (sp, spk)

        def stageB1(u):
            c0, hh = u['c0'], u['hh']
            pc, pck = CUM[hh]
            sp, spk = u['sp']
            et, ek = u['et']
            S.op('pe', lambda e: e.matmul(pc[:, c0:512], lhsT=cumU[:, :], rhs=sp[:, c0:512], start=u['first'],
                                          stop=False, skip_group_check=True),
                 reads=[spk, 'cumU_b'], writes=[pck])
            t2, t2k = t2r.next()
            S.op('dve', lambda e: e.tensor_tensor(out=t2[:, c0:512], in0=pc[:, c0:512], in1=et[:, c0:512],
                                                  op=ALU.subtract),
                 reads=[pck, ek], writes=[t2k])
            u['t2'] = (t2, t2k)

        def stageB2(u):
            c0 = u['c0']
            t2, t2k = u['t2']
            wt, wk = wr.next()
            S.op('act', lambda e: e.activation(out=wt[:, c0:512], in_=t2[:, c0:512], func=AF.Exp),
                 reads=[t2k], writes=[wk])
            if u['diag']:
                S.op('pool', lambda e: e.tensor_tensor(out=wt[:, c0:c0 + 128], in0=wt[:, c0:c0 + 128],
                                                       in1=strict_b[:, :], op=ALU.mult),
                     reads=[wk, 'strict_b'], writes=[wk])
            u['wt'] = (wt, wk)

        def stageC(u, which):
            c0, hh, pr, kb, g = u['c0'], u['hh'], u['pr'], u['kb'], u['g']
            pc, pck = CUM[hh]
            sp, spk = u['sp']
            wt, wk = u['wt']
            po, pok = OUT
            if which == 1:
                if not u['last']:
                    S.op('pe', lambda e: e.matmul(pc[:, c0:512], lhsT=cumL[:, :], rhs=sp[:, c0:512], start=False,
                                                  stop=False, skip_group_check=True),
                         reads=[spk, 'cumL_b'], writes=[pck])
                return
            hd = 2 * pr + hh
            S.op('pe', lambda e: e.matmul(po[:, c0:512], lhsT=Vs[:, kb, hd * 128:(hd + 1) * 128],
                                          rhs=wt[:, c0:512], start=(u['first'] and hh == 0),
                                          stop=(u['last'] and hh == 1), skip_group_check=True),
                 reads=[wk, ('Vs', kb // 8)], writes=[pok])
            if u['last'] and hh == 1:
                yn, ynk = ynr.next()
                _head_norm_T(S, P, po, pok, gsb[:, pr:pr + 1], 'gsb', yn, ynk, rrr, sqr)
                S.dma('sp', T['mixedT'][pr * 128:(pr + 1) * 128, g * 512:(g + 1) * 512], yn[:, :], reads=[ynk],
                      semkey=ynk)

        saved = P['ps'], P['psi']
        P['ps'] = [saved[0][6]]
        P['psi'] = 0
        n = len(units)
        S.barrier_exclude = {('dma', 'uvconv')}
        nconv = NEXP // 128
        conv_done = [0]
        for i in range(n + 4):
            want = min(nconv, (i * nconv) // max(1, n - 8) + 1)
            while conv_done[0] < want:
                c = conv_done[0]
                S.dma('pool', T['uvb'][c * 128:(c + 1) * 128, :], T['expert_uv'][c * 128:(c + 1) * 128, :],
                      semkey='uvconv')
                conv_done[0] += 1
            if i < n:
                stageA1(units[i])
            if 0 <= i - 1 < n:
                stageA2(units[i - 1])
            if 0 <= i - 4 < n:
                stageC(units[i - 4], 1)
                stageC(units[i - 4], 2)
            if 0 <= i - 2 < n:
                stageB1(units[i - 2])
            if 0 <= i - 3 < n:
                stageB2(units[i - 3])
            if other is not None and i % every == 0:
                next(other, None)
        if other is not None:
            for _ in other:
                pass
        P['ps'] = [saved[0][i] for i in range(7)]
        P['psi'] = saved[1]
        _barrier(S)


def phase_B3(S, T, P, Slen):
    nt = Slen // 128
    identb = P['ident_b']
    psb = P['psb']
    tri = P['tri']
    ones = P['ones']
    LNS = float(np.log(128.0 ** -0.5))
    bk = P['ps_all']
    bA, kA = bk[0]
    bS, kS = bk[1]
    bN = [bk[2], bk[3]]
    bP = [bk[4], bk[5]]
    bX, kX = bk[6]
    with ExitStack() as st:
        tris = S.sb('tris', [128, 128], F32, st)
        S.op('dve', lambda e: e.tensor_scalar(out=tris[:, :], in0=tri[:, :], scalar1=128.0 ** -0.5, scalar2=None,
                                              op0=ALU.mult), reads=['tri'], writes=['tris'])
        gml = S.sb('gml', [128, 512], F32, st)
        S.dma('sp', gml[:, :], T['gml_bc'][:, :], writes=['gml'], semkey='gml')
        C = S.sb('Cst', [128, 4, 129], F32, st)
        Cb = S.sb('Cbf', [128, 4, 129], BF16, st)
        S.op('dve', lambda e: e.memset(C[:, :, :], 0.0), writes=[('C', h) for h in range(4)])
        S.op('dve', lambda e: e.memset(Cb[:, :, :], 0.0), writes=[('Cb', h) for h in range(4)])
        qr = Ring(S, 'qml', 4, [128, 4, 128], BF16, st)
        kr = Ring(S, 'kml', 4, [128, 4, 128], BF16, st)
        vr = Ring(S, 'vaug', 4, [128, 4, 129], BF16, st)
        for t, k in vr.tiles:
            S.op('dve', lambda e, t=t: e.memset(t[:, :, 128:129], 1.0), writes=[k])
        gr = Ring(S, 'gtm', 4, [128, 8], F32, st)
        sor = Ring(S, 'sgo', 4, [128, 512], BF16, st)
        bir = Ring(S, 'bias', 2, [128, 4], F32, st)
        ebr = Ring(S, 'ebias', 2, [128, 4], F32, st)
        lfr = Ring(S, 'lfbc', 4, [128, 128], F32, st)
        eqr = Ring(S, 'EQ', 2, [128, 4, 128], F32, st)
        decr = Ring(S, 'dec', 2, [128, 4], F32, st)
        dtr = Ring(S, 'DT', 2, [128, 4, 128], F32, st)
        dmr = Ring(S, 'DM', 2, [128, 4, 128], F32, st)
        smr = Ring(S, 'SM', 2, [128, 4, 128], BF16, st)
        qgr = Ring(S, 'qg', 2, [128, 4, 128], BF16, st)
        ktr = Ring(S, 'ktok', 2, [128, 4, 128], BF16, st)
        dnr = Ring(S, 'dn', 8, [128, 2], F32, st)
        tcr = Ring(S, 'tmpC', 2, [128, 129], F32, st)
        hmlr = Ring(S, 'hml', 2, [128, 512], F32, st)
        ss4r = Ring(S, 'ss4', 2, [128, 4], F32, st)
        jr = Ring(S, 'junk3', 2, [128, 128], BF16, st)
        ytr = Ring(S, 'yt', 2, [128, 512], F32, st)
        ymr = Ring(S, 'ym', 2, [128, 512], BF16, st)
        ystr = Ring(S, 'yst', 2, [128, 4, 128], BF16, st)
        qv = T['qmlT'].ap().rearrange("(h d) t -> d h t", d=128)
        kv = T['kmlT'].ap().rearrange("(h d) t -> d h t", d=128)
        mv = T['mixedT'].ap()[512:1024, :].rearrange("(h p) t -> p h t", p=128)
        state = {}
        epi_state = {}

        loaded = {}

        def pre_load(i):
            ts_ = slice(i * 128, (i + 1) * 128)
            qt, qk = qr.next()
            kt, kk = kr.next()
            vt, vk = vr.next()
            gt, gk = gr.next()
            so, sok = sor.next()
            S.dma('sp', qt[:, :, :], qv[:, :, ts_], writes=[qk], semkey=qk)
            S.dma('sp', kt[:, :, :], kv[:, :, ts_], writes=[kk], semkey=kk)
            S.dma('sp', vt[:, :, 0:128], T['vml'].ap()[ts_, :].rearrange("p (h d) -> p h d", h=4), writes=[vk], semkey=vk)
            S.dma('sp', gt[:, :], T['gates'][ts_, :], writes=[gk], semkey=gk)
            S.dma('sp', so[:, :], T['sigo'][ts_, :], writes=[sok], semkey=sok)
            loaded[i] = (qt, qk, kt, kk, vt, vk, gt, gk, so, sok)

        def pre(i):
            qt, qk, kt, kk, vt, vk, gt, gk, so, sok = loaded.pop(i)
            S.op('pe', lambda e: e.matmul(bX[:, 0:4], lhsT=tri[:, :], rhs=gt[:, 4:8], start=True, stop=True),
                 reads=['tri', gk], writes=[kX])
            bi, bik = bir.next()
            S.op('dve', lambda e: e.tensor_tensor(out=bi[:, :], in0=gt[:, 0:4], in1=bX[:, 0:4], op=ALU.subtract),
                 reads=[gk, kX], writes=[bik])
            eb, ebk = ebr.next()
            S.op('act', lambda e: e.activation(out=eb[:, :], in_=bi[:, :], func=AF.Exp), reads=[bik], writes=[ebk])
            for hd in range(4):
                lf, lfk = lfr.next()
                S.op('dve', lambda e, hd=hd, lf=lf: e.tensor_scalar(out=lf[:, :], in0=ones[:, :],
                                                                    scalar1=gt[:, 4 + hd:5 + hd], scalar2=None,
                                                                    op0=ALU.mult), reads=['ones', gk], writes=[lfk])
                S.op('pe', lambda e, hd=hd, lf=lf: e.matmul(bA[:, hd * 128:(hd + 1) * 128], lhsT=lf[:, :], rhs=tri[:, :],
                                                            start=True, stop=True, skip_group_check=True),
                     reads=[lfk, 'tri'], writes=[kA])
            for hd in range(4):
                S.op('pe', lambda e, hd=hd: e.matmul(bS[:, hd * 128:(hd + 1) * 128], lhsT=kt[:, hd, :], rhs=qt[:, hd, :],
                                                     start=True, stop=True, skip_group_check=True),
                     reads=[kk, qk], writes=[kS])
            for hd in range(4):
                S.op('pe', lambda e, hd=hd: e.transpose(out=psb[:, hd * 128:(hd + 1) * 128], in_=kt[:, hd, :],
                                                        identity=identb[:, :]), reads=[kk, 'ident_b'], writes=['psb'])
            eq, eqk = eqr.next()
            S.op('act', lambda e: e.activation(out=eq[:, :, :], in_=bA[:, :].rearrange("p (h t) -> p h t", h=4),
                                               func=AF.Exp, bias=LNS), reads=[kA], writes=[eqk])
            dec, deck = decr.next()
            S.op('act', lambda e: e.activation(out=dec[:, :], in_=bA[:, :].rearrange("p (h t) -> p h t", h=4)[:, :, 127],
                                               func=AF.Exp), reads=[kA], writes=[deck])
            dt_, dtk = dtr.next()
            for hd in range(4):
                S.op('act', lambda e, hd=hd: e.activation(out=dt_[:, hd, :], in_=bA[:, hd * 128:(hd + 1) * 128],
                                                          func=AF.Exp, bias=bi[:, hd:hd + 1]),
                     reads=[kA, bik], writes=[dtk])
            ktk_t, ktk = ktr.next()
            for hd in range(4):
                S.op('act', lambda e, hd=hd: e.activation(out=ktk_t[:, hd, :], in_=psb[:, hd * 128:(hd + 1) * 128],
                                                          func=AF.Copy, scale=eb[:, hd:hd + 1]),
                     reads=['psb', ebk], writes=[ktk])
            dm, dmk = dmr.next()
            S.op('pool', lambda e: e.tensor_tensor(out=dm[:, :, :], in0=dt_[:, :, :],
                                                   in1=tris[:, :].unsqueeze(1).to_broadcast([128, 4, 128]), op=ALU.mult),
                 reads=[dtk, 'tris'], writes=[dmk])
            qg, qgk = qgr.next()
            S.op('pool', lambda e: e.tensor_tensor(out=qg[:, :, :], in0=qt[:, :, :], in1=eq[:, :, :], op=ALU.mult),
                 reads=[qk, eqk], writes=[qgk])
            sm, smk = smr.next()
            S.op('dve', lambda e: e.tensor_tensor(out=sm[:, :, :], in0=bS[:, :].rearrange("p (h t) -> p h t", h=4),
                                                  in1=dm[:, :, :], op=ALU.mult), reads=[kS, dmk], writes=[smk])
            state[i] = dict(vt=vt, vk=vk, so=so, sok=sok, dec=dec, deck=deck, sm=sm, smk=smk, qg=qg, qgk=qgk,
                            ktk_t=ktk_t, ktk=ktk)

        def post_pe(i):
            d = state[i]
            vt, vk = d['vt'], d['vk']
            sm, smk, qg, qgk, ktk_t, ktk = d['sm'], d['smk'], d['qg'], d['qgk'], d['ktk_t'], d['ktk']
            for hd in range(4):
                pN, pNk = bN[hd // 2]
                c0 = (hd % 2) * 256
                S.op('pe', lambda e, hd=hd, pN=pN, c0=c0: e.matmul(pN[:, c0:c0 + 129], lhsT=sm[:, hd, :], rhs=vt[:, hd, :],
                                                                   start=True, stop=False, skip_group_check=True),
                     reads=[smk, vk], writes=[pNk])
                S.op('pe', lambda e, hd=hd, pN=pN, c0=c0: e.matmul(pN[:, c0:c0 + 129], lhsT=qg[:, hd, :], rhs=Cb[:, hd, :],
                                                                   start=False, stop=True, skip_group_check=True),
                     reads=[qgk, ('Cb', hd)], writes=[pNk])
            for hd in range(4):
                pP, pPk = bP[hd // 2]
                c0 = (hd % 2) * 256
                S.op('pe', lambda e, hd=hd, pP=pP, c0=c0: e.matmul(pP[:, c0:c0 + 129], lhsT=ktk_t[:, hd, :], rhs=vt[:, hd, :],
                                                                   start=True, stop=True, skip_group_check=True),
                     reads=[ktk, vk], writes=[pPk])

        def post_dve(i):
            d = state[i]
            so, sok, dec, deck = d['so'], d['sok'], d['dec'], d['deck']
            hml, hmk = hmlr.next()
            dns = [dnr.next() for _ in range(4)]
            info = [(bN[hd // 2][0], bN[hd // 2][1], (hd % 2) * 256) for hd in range(4)]
            for hd in range(4):
                pN, pNk, c0 = info[hd]
                dn, dnk = dns[hd]
                S.op('dve', lambda e, pN=pN, c0=c0, dn=dn: e.tensor_scalar(
                    out=dn[:, 0:1], in0=pN[:, c0 + 128:c0 + 129], scalar1=-1.0, scalar2=1.0, op0=ALU.mult, op1=ALU.max),
                    reads=[pNk], writes=[(dnk, 0)])
            for hd in range(4):
                pN, pNk, c0 = info[hd]
                dn, dnk = dns[hd]
                S.op('dve', lambda e, pN=pN, c0=c0, dn=dn: e.tensor_scalar(
                    out=dn[:, 1:2], in0=pN[:, c0 + 128:c0 + 129], scalar1=1.0, scalar2=None, op0=ALU.max),
                    reads=[pNk], writes=[(dnk, 1)])
            for hd in range(4):
                dn, dnk = dns[hd]
                S.op('dve', lambda e, dn=dn: e.tensor_tensor(out=dn[:, 0:1], in0=dn[:, 0:1], in1=dn[:, 1:2], op=ALU.max),
                     reads=[(dnk, 0), (dnk, 1)], writes=[(dnk, 0)])
            for hd in range(4):
                dn, dnk = dns[hd]
                S.op('dve', lambda e, dn=dn: e.reciprocal(out=dn[:, 1:2], in_=dn[:, 0:1]), reads=[(dnk, 0)],
                     writes=[(dnk, 1)])
            for hd in range(4):
                pN, pNk, c0 = info[hd]
                dn, dnk = dns[hd]
                hs = slice(hd * 128, (hd + 1) * 128)
                S.op('dve', lambda e, pN=pN, c0=c0, dn=dn, hs=hs: e.tensor_scalar(
                    out=hml[:, hs], in0=pN[:, c0:c0 + 128], scalar1=dn[:, 1:2], scalar2=None, op0=ALU.mult),
                    reads=[pNk, (dnk, 1)], writes=[(hmk, hd)])
            for hd in range(4):
                pP, pPk = bP[hd // 2]
                c0 = (hd % 2) * 256
                tc_, tck = tcr.next()
                S.op('dve', lambda e, hd=hd, pP=pP, c0=c0, tc_=tc_: e.tensor_tensor(
                    out=tc_[:, :], in0=pP[:, c0:c0 + 129], in1=C[:, hd, :], op=ALU.add),
                    reads=[pPk, ('C', hd)], writes=[tck])
                S.op('dve', lambda e, hd=hd, tc_=tc_: e.tensor_scalar(out=C[:, hd, :], in0=tc_[:, :],
                                                                      scalar1=dec[:, hd:hd + 1], scalar2=None,
                                                                      op0=ALU.mult), reads=[tck, deck], writes=[('C', hd)])
            epi_state[i] = dict(hml=hml, hmk=hmk, so=so, sok=sok)
            state.pop(i)

        def cb(i):
            for hd in range(4):
                S.op('act', lambda e, hd=hd: e.activation(out=Cb[:, hd, :], in_=C[:, hd, :], func=AF.Copy),
                     reads=[('C', hd)], writes=[('Cb', hd)])

        def epi(i):
            ts_ = slice(i * 128, (i + 1) * 128)
            d = epi_state.pop(i)
            hml, hmk, so, sok = d['hml'], d['hmk'], d['so'], d['sok']
            ss4, s4k = ss4r.next()
            for hd in range(4):
                hs = slice(hd * 128, (hd + 1) * 128)
                jk_t, jk = jr.next()
                S.op('act', lambda e, hd=hd, hs=hs, jk_t=jk_t: e.activation(
                    out=jk_t[:, :], in_=hml[:, hs], func=AF.Square, accum_out=ss4[:, hd:hd + 1]),
                    reads=[(hmk, hd)], writes=[jk, (s4k, hd)])
            S.op('act', lambda e: e.activation(out=ss4[:, :], in_=ss4[:, :], func=AF.Ln, scale=1.0 / 128, bias=EPS),
                 reads=[(s4k, h) for h in range(4)], writes=[s4k])
            S.op('act', lambda e: e.activation(out=ss4[:, :], in_=ss4[:, :], func=AF.Exp, scale=-0.5),
                 reads=[s4k], writes=[s4k])
            yt, ytk = ytr.next()
            for hd in range(4):
                hs = slice(hd * 128, (hd + 1) * 128)
                S.op('dve', lambda e, hd=hd, hs=hs: e.scalar_tensor_tensor(
                    out=yt[:, hs], in0=hml[:, hs], scalar=ss4[:, hd:hd + 1], in1=gml[:, hs], op0=ALU.mult, op1=ALU.mult),
                    reads=[(hmk, hd), s4k, 'gml'], writes=[(ytk, hd)])
            ym, ymk = ymr.next()
            S.op('dve', lambda e: e.tensor_tensor(out=ym[:, :], in0=yt[:, :], in1=so[:, :], op=ALU.mult),
                 reads=[(ytk, h) for h in range(4)] + [sok], writes=[ymk])
            epi_state[i] = dict(ym=ym, ymk=ymk)

        def epi_b(i):
            ts_ = slice(i * 128, (i + 1) * 128)
            d = epi_state.pop(i)
            ym, ymk = d['ym'], d['ymk']
            for hd in range(4):
                S.op('pe', lambda e, hd=hd: e.transpose(out=psb[:, 512 + hd * 128:512 + (hd + 1) * 128],
                                                        in_=ym[:, hd * 128:(hd + 1) * 128], identity=identb[:, :]),
                     reads=[ymk, 'ident_b'], writes=['psb'])
            ys, ysk = ystr.next()
            S.op('act', lambda e: e.activation(out=ys[:, :, :], in_=psb[:, 512:1024].rearrange("p (h t) -> p h t", h=4),
                                               func=AF.Copy), reads=['psb'], writes=[ysk])
            S.dma('sp', mv[:, :, ts_], ys[:, :, :], reads=[ysk], semkey=ysk)

        for j in range(min(3, nt)):
            pre_load(j)
        pre(0)
        for i in range(nt + 1):
            if i >= 1:
                epi(i - 1)
            if i < nt:
                post_pe(i)
                post_dve(i)
            if i >= 1:
                epi_b(i - 1)
            if i + 1 < nt:
                pre(i + 1)
            if i < nt:
                cb(i)
            if i + 3 < nt:
                pre_load(i + 3)
        _barrier(S)


def phase_B4(S, T, P, Slen):
    mod = P['mod']
    ng = Slen // 512
    with ExitStack() as st:
        w_out = S.sb('w_out', [128, 8, D], BF16, st)
        wov = T['w_out'].ap().rearrange("(j p) n -> p j n", p=128)
        WO = []
        for j in range(8):
            WO.append(('w_out', j))
            S.dma('pool', w_out[:, j, :], wov[:, j, :], writes=[('w_out', j)], semkey='w_out')
        mr = Ring(S, 'mT', 2, [128, 8, 512], BF16, st)
        xr = Ring(S, 'xo', 3, [128, D], F32, st)
        tr = Ring(S, 'to', 2, [128, D], F32, st)
        mview = T['mixedT'].ap().rearrange("(j p) t -> p j t", p=128)
        for g in range(ng):
            mt, mk = mr.next()
            S.dma('sp', mt[:, :, :], mview[:, :, g * 512:(g + 1) * 512], writes=[mk], semkey=mk)
            for tl in range(4):
                t0 = g * 512 + tl * 128
                xt, xk = xr.next()
                S.dma('sp', xt[:, :], T['x'][t0:t0 + 128, :], writes=[xk], semkey=xk)
                tt, tk = tr.next()
                for half in range(2):
                    pst, pk = _psnext(P)
                    cs = slice(half * 512, (half + 1) * 512)
                    for j in range(8):
                        S.op('pe', lambda e, j=j: e.matmul(pst[:, :], lhsT=mt[:, j, tl * 128:(tl + 1) * 128],
                                                           rhs=w_out[:, j, cs], start=(j == 0), stop=(j == 7)),
                             reads=WO + [mk], writes=[pk])
                    S.op('dve', lambda e: e.tensor_tensor(out=tt[:, cs], in0=pst[:, :], in1=mod[:, 2048 + half * 512:2048 + (half + 1) * 512],
                                                          op=ALU.mult), reads=[pk, 'mod'], writes=[tk])
                S.op('dve', lambda e: e.tensor_tensor(out=xt[:, :], in0=tt[:, :], in1=xt[:, :], op=ALU.add),
                     reads=[tk, xk], writes=[xk])
                S.dma('sp', T['x1'][t0:t0 + 128, :], xt[:, :], reads=[xk], semkey=('x1o', xk))
        _barrier(S)


def phase_C(S, T, P, Slen):
    mod = P['mod']
    nt = Slen // 128
    identb = P['ident_b']
    psb = P['psb']
    S.barrier_exclude = ()
    _barrier(S)
    KB = 2
    with ExitStack() as st:
        w_q, WQ, keys, KEYS = P['w_q'], P['WQ'], P['keys'], P['KEYS']
        iota_i = S.sb('iota_i', [128, 16], I32, st)
        iota_f = S.sb('iota_f', [128, 16], F32, st)
        S.op('pool', lambda e: e.iota(iota_i[:, :], pattern=[[1, 16]], base=0, channel_multiplier=0),
             writes=['iota_i'])
        S.op('dve', lambda e: e.tensor_copy(out=iota_f[:, :], in_=iota_i[:, :]), reads=['iota_i'], writes=['iota_f'])
        xr = Ring(S, 'x1t', 2, [128, D], F32, st)
        h2r = Ring(S, 'h2', 1, [128, D], F32, st)
        junk = S.sb('junkc', [128, D], BF16, st)
        ssr = Ring(S, 'ssc', 2, [128, 1], F32, st)
        hbr = Ring(S, 'hbc', 2, [128, D], BF16, st)
        pdr = Ring(S, 'pd', 3, [128, D], BF16, st)
        h2T = S.sb('h2T', [128, 8, 128], BF16, st)
        qTs = S.sb('qTs', [128, 16, 128], BF16, st)
        sc = S.sb('sc', [128, 16, 128], F32, st)
        scr8 = S.sb('scr8', [128, 2048], F32, st)
        sc2 = scr8[:, :].rearrange('p (a b) -> p a b', a=16)
        tv = S.sb('tv', [128, 16, 16], F32, st)
        ti = S.sb('ti', [128, 16, 16], U32, st)
        tif = S.sb('tif', [128, 16, 16], F32, st)
        cand = S.sb('cand', [128, 8, 256], F32, st)
        cand2 = scr8[:, :].rearrange('p (a b) -> p a b', a=8)
        bv = S.sb('bv', [128, 8, 16], F32, st)
        bp = S.sb('bp', [128, 8, 16], U32, st)
        au = S.sb('au', [128, 8, 16], U32, st)
        bu = S.sb('bu', [128, 8, 16], U32, st)
        af = S.sb('af', [128, 8, 16], F32, st)
        bf = S.sb('bf', [128, 8, 16], F32, st)
        oh = scr8[:, :].rearrange('p (a b c) -> p a b c', a=8, b=16)
        i1 = S.sb('i1', [128, 8, 16], F32, st)
        i2 = S.sb('i2', [128, 8, 16], F32, st)
        ef = S.sb('ef', [128, 128], F32, st)
        idxr = Ring(S, 'idx', 2, [128, 128], I32, st)
        nmax = S.sb('nmax', [128, 8], F32, st)
        gexp = S.sb('gexp', [128, 8, 16], F32, st)
        gsum = S.sb('gsum', [128, 8], F32, st)
        gater = Ring(S, 'gate', 2, [128, 8, 16], F32, st)
        prer = Ring(S, 'pre', 2, [128, 128], F32, st)
        wgr = Ring(S, 'wg', 2, [128, 128], F32, st)
        wgtr = Ring(S, 'wgt', 2, [128, 128], F32, st)
        dgr = Ring(S, 'dg', 4, [128, 128], BF16, st)
        ur = Ring(S, 'UV', 16, [128, 2 * D], BF16, st)
        yr = Ring(S, 'yacc', 1, [128, D], F32, st)
        tv4 = tv[:, :, :].rearrange("p (h two) k -> p h two k", two=2)
        tif4 = tif[:, :, :].rearrange("p (h two) k -> p h two k", two=2)
        cand4 = cand[:, :, :].rearrange("p h (a b) -> p h a b", a=16)
        saved = P['ps'], P['psi']
        allps = saved[0]
        P['ps'] = [allps[4], allps[5], allps[6]]
        P['psi'] = 0
        tile_state = {}

        def front(i):
            ts_ = slice(i * 128, (i + 1) * 128)
            xt, xk = xr.next()
            S.dma('sp', xt[:, :], T['x1'][ts_, :], writes=[xk], semkey=xk)
            yield
            yield
            ss, sk = ssr.next()
            S.op('act', lambda e: e.activation(out=junk[:, :], in_=xt[:, :], func=AF.Square, accum_out=ss[:, :]),
                 reads=[xk], writes=['junkc', sk])
            S.op('act', lambda e: e.activation(out=ss[:, :], in_=ss[:, :], func=AF.Ln, scale=1.0 / D, bias=EPS),
                 reads=[sk], writes=[sk])
            S.op('act', lambda e: e.activation(out=ss[:, :], in_=ss[:, :], func=AF.Exp, scale=-0.5),
                 reads=[sk], writes=[sk])
            yield
            h2, hk = h2r.next()
            S.op('dve', lambda e: e.scalar_tensor_tensor(out=h2[:, :], in0=xt[:, :], scalar=ss[:, 0:1],
                                                         in1=mod[:, 4096:5120], op0=ALU.mult, op1=ALU.mult),
                 reads=[xk, sk, 'mod'], writes=[hk])
            hb, hbk = hbr.next()
            S.op('dve', lambda e: e.tensor_tensor(out=hb[:, :], in0=h2[:, :], in1=mod[:, 3072:4096], op=ALU.add),
                 reads=[hk, 'mod'], writes=[hbk])
            yield
            for j in range(8):
                S.op('pe', lambda e, j=j: e.transpose(out=psb[:, j * 128:(j + 1) * 128], in_=hb[:, j * 128:(j + 1) * 128],
                                                      identity=identb[:, :]), reads=[hbk, 'ident_b'], writes=['psb'])
            yield
            S.op('act', lambda e: e.activation(out=h2T[:, :, :], in_=psb[:, :].rearrange("p (j t) -> p j t", j=8),
                                               func=AF.Copy), reads=['psb'], writes=['h2T'])
            yield
            pend = None
            for q4 in range(4):
                pst, pk = _psnext(P)
                for hpi in range(4):
                    hp = q4 * 4 + hpi
                    for j in range(8):
                        S.op('pe', lambda e, j=j, hp=hp, hpi=hpi, pst=pst: e.matmul(
                            pst[:, hpi * 128:(hpi + 1) * 128], lhsT=w_q[:, j, hp * 128:(hp + 1) * 128], rhs=h2T[:, j, :],
                            start=(j == 0), stop=(j == 7), skip_group_check=True), reads=WQ + ['h2T'], writes=[pk])
                    if hpi == 0 and pend is not None:
                        pend()
                        pend = None
                    if hpi % 2 == 1:
                        yield
                pend = (lambda pst=pst, pk=pk, q4=q4: S.op('act', lambda e: e.activation(
                    out=qTs[:, q4 * 4:(q4 + 1) * 4, :], in_=pst[:, :].rearrange("p (a t) -> p a t", a=4), func=AF.Copy),
                    reads=[pk], writes=[('qTs', q4)]))
            pend()
            yield
            pend = None
            for q4 in range(4):
                pst, pk = _psnext(P)
                for hpi in range(4):
                    hp = q4 * 4 + hpi
                    S.op('pe', lambda e, hp=hp, hpi=hpi, pst=pst: e.matmul(
                        pst[:, hpi * 128:(hpi + 1) * 128], lhsT=qTs[:, hp, :], rhs=keys[:, hp, :], start=True, stop=True,
                        skip_group_check=True), reads=[('qTs', q4)] + KEYS, writes=[pk])
                if pend is not None:
                    pend()
                pend = (lambda pst=pst, pk=pk, q4=q4: S.op('act', lambda e: e.activation(
                    out=sc[:, q4 * 4:(q4 + 1) * 4, :], in_=pst[:, :].rearrange("p (a t) -> p a t", a=4), func=AF.Copy),
                    reads=[pk], writes=[('sc', q4)]))
                yield
            pend()
            yield
            for g4 in range(4):
                hps = [g4 * 4 + q_ for q_ in range(4)]
                sck = ('sc', g4)
                for hp in hps:
                    S.op('dve', lambda e, hp=hp: e.max(out=tv[:, hp, 0:8], in_=sc[:, hp, :]), reads=[sck], writes=[('tv', hp)])
                for hp in hps:
                    S.op('dve', lambda e, hp=hp: e.max_index(out=ti[:, hp, 0:8], in_max=tv[:, hp, 0:8], in_values=sc[:, hp, :]),
                         reads=[sck, ('tv', hp)], writes=[('ti', hp)])
                for hp in hps:
                    S.op('dve', lambda e, hp=hp: e.match_replace(out=sc2[:, hp, :], in_to_replace=tv[:, hp, 0:8],
                                                                 in_values=sc[:, hp, :], imm_value=-1e30),
                         reads=[sck, ('tv', hp)], writes=[('scr8', hp)])
                yield
                for hp in hps:
                    S.op('dve', lambda e, hp=hp: e.max(out=tv[:, hp, 8:16], in_=sc2[:, hp, :]), reads=[('scr8', hp)],
                         writes=[('tv', hp)])
                for hp in hps:
                    S.op('dve', lambda e, hp=hp: e.max_index(out=ti[:, hp, 8:16], in_max=tv[:, hp, 8:16],
                                                             in_values=sc2[:, hp, :]), reads=[('scr8', hp), ('tv', hp)],
                         writes=[('ti', hp)])
                yield
            S.op('dve', lambda e: e.tensor_copy(out=tif[:, :, :], in_=ti[:, :, :]), reads=[('ti', q_) for q_ in range(16)], writes=['tif'])
            S.op('dve', lambda e: e.tensor_tensor(
                out=cand4, in0=tv4[:, :, 0:1, :].rearrange("p h o k -> p h k o").to_broadcast([128, 8, 16, 16]),
                in1=tv4[:, :, 1:2, :].to_broadcast([128, 8, 16, 16]), op=ALU.add), reads=[('tv', q_) for q_ in range(16)], writes=['cand'])
            yield
            for g4 in range(2):
                hs_ = [g4 * 4 + q_ for q_ in range(4)]
                for h in hs_:
                    S.op('dve', lambda e, h=h: e.max(out=bv[:, h, 0:8], in_=cand[:, h, :]), reads=['cand'], writes=[('bv', h)])
                for h in hs_:
                    S.op('dve', lambda e, h=h: e.max_index(out=bp[:, h, 0:8], in_max=bv[:, h, 0:8], in_values=cand[:, h, :]),
                         reads=['cand', ('bv', h)], writes=[('bp', h)])
                for h in hs_:
                    S.op('dve', lambda e, h=h: e.match_replace(out=cand2[:, h, :], in_to_replace=bv[:, h, 0:8],
                                                               in_values=cand[:, h, :], imm_value=-1e30),
                         reads=['cand', ('bv', h)], writes=[('scr8', h)])
                yield
                for h in hs_:
                    S.op('dve', lambda e, h=h: e.max(out=bv[:, h, 8:16], in_=cand2[:, h, :]), reads=[('scr8', h)],
                         writes=[('bv', h)])
                for h in hs_:
                    S.op('dve', lambda e, h=h: e.max_index(out=bp[:, h, 8:16], in_max=bv[:, h, 8:16],
                                                           in_values=cand2[:, h, :]), reads=[('scr8', h), ('bv', h)],
                         writes=[('bp', h)])
                yield
            S.op('dve', lambda e: e.tensor_scalar(out=nmax[:, :], in0=bv[:, :, 0], scalar1=-1.0, scalar2=None,
                                                  op0=ALU.mult), reads=[('bv', q_) for q_ in range(8)], writes=['nmax'])
            yield
            for h in range(8):
                S.op('act', lambda e, h=h: e.activation(out=gexp[:, h, :], in_=bv[:, h, :], func=AF.Exp,
                                                        bias=nmax[:, h:h + 1], accum_out=gsum[:, h:h + 1]),
                     reads=[('bv', q_) for q_ in range(8)] + ['nmax'], writes=['gexp', 'gsum'])
            yield
            S.op('dve', lambda e: e.reciprocal(out=gsum[:, :], in_=gsum[:, :]), reads=['gsum'], writes=['gsum'])
            gate, gak = gater.next()
            S.op('dve', lambda e: e.tensor_tensor(out=gate[:, :, :], in0=gexp[:, :, :],
                                                  in1=gsum[:, :].unsqueeze(2).to_broadcast([128, 8, 16]), op=ALU.mult),
                 reads=['gexp', 'gsum'], writes=[gak])
            yield
            S.op('dve', lambda e: e.tensor_single_scalar(out=au[:, :, :], in_=bp[:, :, :], scalar=4,
                                                         op=ALU.logical_shift_right), reads=[('bp', q_) for q_ in range(8)], writes=['au'])
            S.op('dve', lambda e: e.tensor_single_scalar(out=bu[:, :, :], in_=bp[:, :, :], scalar=15,
                                                         op=ALU.bitwise_and), reads=[('bp', q_) for q_ in range(8)], writes=['bu'])
            S.op('dve', lambda e: e.tensor_copy(out=af[:, :, :], in_=au[:, :, :]), reads=['au'], writes=['af'])
            S.op('dve', lambda e: e.tensor_copy(out=bf[:, :, :], in_=bu[:, :, :]), reads=['bu'], writes=['bf'])
            yield
            iota_b = iota_f[:, :].unsqueeze(1).unsqueeze(1).to_broadcast([128, 8, 16, 16])
            for src, half, dst, dk in ((af, 0, i1, 'i1'), (bf, 1, i2, 'i2')):
                S.op('dve', lambda e, src=src: e.tensor_tensor(
                    out=oh[:, :, :, :], in0=src[:, :, :].unsqueeze(3).to_broadcast([128, 8, 16, 16]), in1=iota_b,
                    op=ALU.is_equal), reads=['af', 'bf', 'iota_f'], writes=['scr8'] + [('scr8', q_) for q_ in range(16)])
                yield
                S.op('dve', lambda e, half=half: e.tensor_tensor(
                    out=oh[:, :, :, :], in0=oh[:, :, :, :],
                    in1=tif4[:, :, half:half + 1, :].to_broadcast([128, 8, 16, 16]), op=ALU.mult),
                    reads=['scr8', 'tif'], writes=['scr8'] + [('scr8', q_) for q_ in range(16)])
                yield
                S.op('dve', lambda e, dst=dst: e.tensor_reduce(out=dst[:, :, :], in_=oh[:, :, :, :], axis=AX.X, op=ALU.add),
                     reads=['scr8'], writes=[dk])
                yield
            S.op('dve', lambda e: e.scalar_tensor_tensor(out=ef[:, :], in0=i1[:, :, :].rearrange("p h k -> p (h k)"),
                                                         scalar=128.0, in1=i2[:, :, :].rearrange("p h k -> p (h k)"),
                                                         op0=ALU.mult, op1=ALU.add), reads=['i1', 'i2'], writes=['ef'])
            idx, idk = idxr.next()
            S.op('dve', lambda e: e.tensor_copy(out=idx[:, :], in_=ef[:, :]), reads=['ef'], writes=[idk])
            tile_state[i] = dict(xt=xt, xk=xk, h2=h2, hk=hk, idx=idx, idk=idk, gate=gate, gak=gak, hbt=hb, hbk=hbk)
            yield

        def back(i, gen):
            ts_ = slice(i * 128, (i + 1) * 128)
            stt = tile_state.pop(i)
            xt, xk, h2, hk, idx, idk, gate, gak = (stt[n] for n in ('xt', 'xk', 'h2', 'hk', 'idx', 'idk', 'gate', 'gak'))
            hbt, hbk = stt['hbt'], stt['hbk']
            gflat = gate[:, :, :].rearrange("p h k -> p (h k)")
            pre, prk = prer.next()
            wg, wgk = wgr.next()
            wgt, wtk = wgtr.next()
            yb = [allps[(i % 2) * 2], allps[(i % 2) * 2 + 1]]
            batch = {}

            def stage1(k0):
                slots = []
                for k in range(k0, k0 + KB):
                    ut, uk = ur.next()
                    slots.append((ut, uk))
                    S.dma('pool', None, None, reads=[idk], writes=[uk], semkey=uk,
                          fn=lambda e, k=k, ut=ut: e.indirect_dma_start(
                              out=ut[:, :], out_offset=None, in_=T['uvb'][:, :],
                              in_offset=bass.IndirectOffsetOnAxis(ap=idx[:, k:k + 1], axis=0)))
                    pd, pdk = pdr.next()
                    S.op('dve', lambda e, ut=ut, pd=pd: e.tensor_tensor(out=pd[:, :], in0=ut[:, 0:D], in1=hbt[:, :],
                                                                        op=ALU.mult), reads=[uk, hbk], writes=[pdk])
                    S.op('act', lambda e, k=k, pd=pd: e.activation(out=pd[:, :], in_=pd[:, :], func=AF.Copy,
                                                                   accum_out=pre[:, k:k + 1]),
                         reads=[pdk], writes=[pdk, (prk, k, 'p')])
                ks = slice(k0, k0 + KB)
                S.op('act', lambda e: e.activation(out=wg[:, ks], in_=pre[:, ks], func=AF.Gelu),
                     reads=[(prk, k, 'p') for k in range(k0, k0 + KB)], writes=[(wgk, k0)])
                batch[k0] = slots

            def stage2(k0):
                slots = batch.pop(k0)
                ks = slice(k0, k0 + KB)
                S.op('dve', lambda e: e.tensor_tensor(out=wgt[:, ks], in0=wg[:, ks], in1=gflat[:, ks], op=ALU.mult),
                     reads=[(wgk, k0), gak], writes=[(wtk, k0)])
                for k in range(k0, k0 + KB):
                    ut, uk = slots[k - k0]
                    dg, dgk = dgr.next()
                    S.op('dve', lambda e, k=k, dg=dg: e.tensor_scalar(out=dg[:, :], in0=identb[:, :],
                                                                      scalar1=wgt[:, k:k + 1], scalar2=None, op0=ALU.mult),
                         reads=['ident_b', (wtk, k0)], writes=[dgk])
                    for half in range(2):
                        S.op('pe', lambda e, k=k, dg=dg, ut=ut, half=half: e.matmul(
                            yb[half][0][:, :], lhsT=dg[:, :], rhs=ut[:, D + half * 512:D + (half + 1) * 512],
                            start=(k == 0), stop=(k == 127)), reads=[dgk, uk], writes=[yb[half][1]])

            SKEW = 2
            for b_ in range(SKEW):
                stage1(b_ * KB)
            for k0 in range(0, 128, KB):
                if k0 + SKEW * KB < 128:
                    stage1(k0 + SKEW * KB)
                stage2(k0)
                if gen is not None:
                    if KB >= 2 or (k0 % 2 == 0):
                        for _ in range(2 if KB >= 4 else 1):
                            next(gen, None)
            if gen is not None:
                for _ in gen:
                    pass
            ya, yk = yr.next()
            for half in range(2):
                cs = slice(half * 512, (half + 1) * 512)
                S.op('dve', lambda e, half=half, cs=cs: e.tensor_tensor(
                    out=ya[:, cs], in0=yb[half][0][:, :], in1=mod[:, 5120 + half * 512:5120 + (half + 1) * 512],
                    op=ALU.mult), reads=[yb[half][1], 'mod'], writes=[yk])
            S.op('dve', lambda e: e.tensor_tensor(out=ya[:, :], in0=ya[:, :], in1=xt[:, :], op=ALU.add),
                 reads=[yk, xk], writes=[yk])
            S.dma('sp', T['out'][ts_, :], ya[:, :], reads=[yk], semkey=('outd', yk))

        for _ in front(0):
            pass
        for i in range(nt):
            gen = front(i + 1) if i + 1 < nt else None
            back(i, gen)
        P['ps'] = [allps[i] for i in range(7)]
        P['psi'] = saved[1]
        _barrier(S)


_NC_CACHE = {}


def kernel(**inputs):
    Slen = 4096
    if 'nc' not in _NC_CACHE:
        _NC_CACHE['nc'] = build(Slen, debug=False, phases="AB234C")
    nc = _NC_CACHE['nc']
    in_maps = [host_inputs(b, inputs, Slen) for b in range(8)]
    res = run_bass_kernel_spmd(nc, in_maps, core_ids=list(range(8)))
    out = np.stack([np.asarray(r["out"], dtype=np.float32) for r in res.results], axis=0)
    return out
```
